# Optimizing a Trainium2 kernel written in Bass

```python
import jax
import jax.numpy as jnp
from jax import lax
import numpy as np


D_MODEL = 1024
BATCH = 8
SEQ = 4096
DEPTH = 4

HEAD_DIM = 64
MLA_HEADS = 6
MLA_NOPE = 64
MLA_ROPE = 32
MLA_V = 64
MLA_Q_RANK = 256
MLA_KV_RANK = 128
ROPE_THETA = 10000.0
MLA_Q_BLOCK = 128
MOBA_HEADS = 6
MOBA_BLOCK = 256
MOBA_TOPK = 3
MOBA_Q_CHUNK = 32
DIL_PATTERNS = ((128, 1), (512, 4), (2048, 16))
DIL_GROUP_HEADS = 4
DIL_HEADS = DIL_GROUP_HEADS * len(DIL_PATTERNS)
N_ALIBI = DIL_HEADS + MOBA_HEADS
N_BRANCHES = 3
D_FF = 2816
NORM_EPS = 1e-6
NEG_INF = -1e30
MLA_IN = MLA_Q_RANK + MLA_KV_RANK + MLA_ROPE
MOBA_IN = 3 * MOBA_HEADS * HEAD_DIM
DIL_IN = 3 * DIL_HEADS * HEAD_DIM
GATE_IN = N_BRANCHES * D_MODEL
IN_COLS = MLA_IN + MOBA_IN + DIL_IN + GATE_IN
MLA_OUT = MLA_HEADS * MLA_V
MOBA_OUT = MOBA_HEADS * HEAD_DIM
DIL_OUT = DIL_GROUP_HEADS * HEAD_DIM

kernel_name = 'hybrid_mla_moba_dilated_block'


def rms_norm(x, gain):
    xf = x.astype(jnp.float32)
    y = xf * lax.rsqrt(jnp.mean(xf * xf, axis=-1, keepdims=True) + NORM_EPS)
    return (y * gain.astype(jnp.float32)).astype(x.dtype)


def swiglu(h, w_gate, w_up, w_down):
    a = jnp.einsum('bsd,df->bsf', h, w_gate)
    u = jnp.einsum('bsd,df->bsf', h, w_up)
    return jnp.einsum('bsf,fd->bsd', jax.nn.silu(a) * u, w_down)


def alibi_slopes():
    return 2.0 ** (-8.0 * jnp.arange(1, N_ALIBI + 1, dtype=jnp.float32) / N_ALIBI)


def rope_tables(seq):
    pos = jnp.arange(seq, dtype=jnp.float32)
    inv = ROPE_THETA ** (-jnp.arange(0, MLA_ROPE, 2, dtype=jnp.float32) / MLA_ROPE)
    ang = pos[:, None] * inv[None, :]
    return jnp.cos(ang), jnp.sin(ang)


def apply_rope(t, cos, sin):
    cos = cos.astype(t.dtype)
    sin = sin.astype(t.dtype)
    t1, t2 = jnp.split(t, 2, axis=-1)
    return jnp.concatenate([t1 * cos - t2 * sin, t1 * sin + t2 * cos], axis=-1)


def mla_attention(h_cq, h_ckv, h_krope, q_norm, w_uq, kv_norm, w_ukv):
    B, S, _ = h_cq.shape
    cos, sin = rope_tables(S)
    c_q = rms_norm(h_cq, q_norm)
    q = jnp.einsum('bsr,rk->bsk', c_q, w_uq).reshape(B, S, MLA_HEADS, MLA_NOPE + MLA_ROPE)
    q = jnp.concatenate([q[..., :MLA_NOPE], apply_rope(q[..., MLA_NOPE:], cos[:, None], sin[:, None])], axis=-1)
    c_kv = rms_norm(h_ckv, kv_norm)
    kv = jnp.einsum('bsr,rk->bsk', c_kv, w_ukv).reshape(B, S, MLA_HEADS, MLA_NOPE + MLA_V)
    k_rope = apply_rope(h_krope, cos, sin)
    k = jnp.concatenate([kv[..., :MLA_NOPE], jnp.broadcast_to(k_rope[:, :, None, :], (B, S, MLA_HEADS, MLA_ROPE))], axis=-1)
    v = kv[..., MLA_NOPE:]
    q, k, v = (t.transpose(0, 2, 1, 3) for t in (q, k, v))
    scale = (MLA_NOPE + MLA_ROPE) ** -0.5
    nb = S // MLA_Q_BLOCK
    q_blocks = q.reshape(B, MLA_HEADS, nb, MLA_Q_BLOCK, MLA_NOPE + MLA_ROPE).transpose(2, 0, 1, 3, 4)
    kpos = jnp.arange(S)

    def block(args):
        qi, i = args
        s = jnp.einsum('bhqd,bhkd->bhqk', qi, k).astype(jnp.float32) * scale
        qpos = i * MLA_Q_BLOCK + jnp.arange(MLA_Q_BLOCK)
        s = jnp.where(kpos[None, :] <= qpos[:, None], s, NEG_INF)
        p = jax.nn.softmax(s, axis=-1).astype(v.dtype)
        return jnp.einsum('bhqk,bhkd->bhqd', p, v)

    o = lax.map(block, (q_blocks, jnp.arange(nb)))
    return o.transpose(1, 0, 3, 2, 4).reshape(B, S, MLA_OUT)


def moba_attention(q, k, v, slopes):
    B, H, S, D = q.shape
    nb = -(-S // MOBA_BLOCK)
    sp = nb * MOBA_BLOCK
    pad = ((0, 0), (0, 0), (0, sp - S), (0, 0))
    qp, kp, vp = (jnp.pad(t, pad) for t in (q, k, v))
    kb = kp.reshape(B, H, nb, MOBA_BLOCK, D)
    vb = vp.reshape(B, H, nb, MOBA_BLOCK, D)
    k_mean = jnp.mean(kb, axis=3)
    k_sel_n = min(MOBA_TOPK, nb)
    scale = D ** -0.5
    bi = jnp.arange(B)[:, None, None, None]
    hi = jnp.arange(H)[None, :, None, None]
    blk_ids = jnp.arange(nb)
    offs = jnp.arange(MOBA_BLOCK)
    slope5 = slopes[None, :, None, None, None]

    def chunk(c):
        start = c * MOBA_Q_CHUNK
        qc = lax.dynamic_slice_in_dim(qp, start, MOBA_Q_CHUNK, axis=2)
        b_own = start // MOBA_BLOCK
        tpos = start + jnp.arange(MOBA_Q_CHUNK)
        gate = jnp.einsum('bhqd,bhnd->bhqn', qc, k_mean).astype(jnp.float32)
        gate = jnp.where(blk_ids < b_own, gate, NEG_INF)
        _, idx = lax.top_k(gate, k_sel_n)
        sel_ok = idx < b_own
        k_sel = kb[bi, hi, idx]
        v_sel = vb[bi, hi, idx]
        s_sel = jnp.einsum('bhqd,bhqnpd->bhqnp', qc, k_sel).astype(jnp.float32) * scale
        kpos_sel = idx[..., None] * MOBA_BLOCK + offs
        dist_sel = (tpos[None, None, :, None, None] - kpos_sel).astype(jnp.float32)
        s_sel = jnp.where(sel_ok[..., None], s_sel - slope5 * dist_sel, NEG_INF)
        k_own = lax.dynamic_index_in_dim(kb, b_own, axis=2, keepdims=False)
        v_own = lax.dynamic_index_in_dim(vb, b_own, axis=2, keepdims=False)
        kpos_own = b_own * MOBA_BLOCK + offs
        dist_own = (tpos[:, None] - kpos_own[None, :]).astype(jnp.float32)
        s_own = jnp.einsum('bhqd,bhpd->bhqp', qc, k_own).astype(jnp.float32) * scale
        s_own = jnp.where(dist_own >= 0, s_own - slopes[None, :, None, None] * dist_own, NEG_INF)
        n_sel = k_sel_n * MOBA_BLOCK
        s = jnp.concatenate([s_sel.reshape(B, H, MOBA_Q_CHUNK, n_sel), s_own], axis=-1)
        p = jax.nn.softmax(s, axis=-1).astype(v.dtype)
        p_sel = p[..., :n_sel].reshape(B, H, MOBA_Q_CHUNK, k_sel_n, MOBA_BLOCK)
        p_own = p[..., n_sel:]
        return (jnp.einsum('bhqnp,bhqnpd->bhqd', p_sel, v_sel)
                + jnp.einsum('bhqp,bhpd->bhqd', p_own, v_own))

    o = lax.map(chunk, jnp.arange(sp // MOBA_Q_CHUNK))
    return o.transpose(1, 0, 3, 2, 4).reshape(B, sp, H * D)[:, :S]


def dilated_group_attention(q, k, v, window, dilation, slopes):
    B, H, S, D = q.shape
    L = window // dilation
    span = dilation * L
    P = -(-S // span) * span
    M = P // dilation
    nbk = M // L

    def to_blocks(t):
        t = jnp.pad(t, ((0, 0), (0, 0), (0, P - S), (0, 0)))
        return t.reshape(B, H, M, dilation, D).transpose(0, 1, 3, 2, 4).reshape(B, H, dilation, nbk, L, D)

    def window_keys(t):
        prev = jnp.pad(t[:, :, :, :-1], ((0, 0), (0, 0), (0, 0), (1, 0), (0, 0), (0, 0)))
        return jnp.concatenate([prev, t], axis=4)

    qb = to_blocks(q)
    kw = window_keys(to_blocks(k))
    vw = window_keys(to_blocks(v))
    s = jnp.einsum('bhrnqd,bhrnkd->bhrnqk', qb, kw).astype(jnp.float32) * (D ** -0.5)
    steps = L + jnp.arange(L)[:, None] - jnp.arange(2 * L)[None, :]
    band = (steps >= 0) & (steps <= L)
    before_start = (jnp.arange(nbk)[:, None, None] == 0) & (jnp.arange(2 * L) < L)[None, None, :]
    valid = band[None] & ~before_start
    dist = (steps * dilation).astype(jnp.float32)
    s = jnp.where(valid, s - slopes[None, :, None, None, None, None] * dist, NEG_INF)
    m = jnp.max(s, axis=-1, keepdims=True)
    e = jnp.exp(s - m)
    den = jnp.sum(e, axis=-1)
    o = jnp.einsum('bhrnqk,bhrnkd->bhrnqd', (e / den[..., None]).astype(v.dtype), vw)
    lse = m[..., 0] + jnp.log(den)
    o = o.reshape(B, H, dilation, M, D).transpose(0, 1, 3, 2, 4).reshape(B, H, P, D)[:, :, :S]
    lse = lse.reshape(B, H, dilation, M).transpose(0, 1, 3, 2).reshape(B, H, P)[:, :, :S]
    return o, lse


def hybrid_mixer(h, w_in, q_norm, w_uq, kv_norm, w_ukv, w_br_mla, w_br_moba, w_br_dil, w_out):
    B, S, _ = h.shape
    proj = jnp.einsum('bsd,dk->bsk', h, w_in)
    i0 = MLA_Q_RANK
    i1 = i0 + MLA_KV_RANK
    i2 = i1 + MLA_ROPE
    i3 = i2 + MOBA_IN
    i4 = i3 + DIL_IN
    h_cq, h_ckv, h_kr, h_moba, h_dil, h_gate = jnp.split(proj, [i0, i1, i2, i3, i4], axis=-1)
    slopes = alibi_slopes()
    a = mla_attention(h_cq, h_ckv, h_kr, q_norm, w_uq, kv_norm, w_ukv)
    mqkv = h_moba.reshape(B, S, 3, MOBA_HEADS, HEAD_DIM).transpose(2, 0, 3, 1, 4)
    b = moba_attention(mqkv[0], mqkv[1], mqkv[2], slopes[DIL_HEADS:])
    dqkv = h_dil.reshape(B, S, 3, DIL_HEADS, HEAD_DIM).transpose(2, 0, 3, 1, 4)
    outs, lses = [], []
    for g, (window, dilation) in enumerate(DIL_PATTERNS):
        hs = slice(g * DIL_GROUP_HEADS, (g + 1) * DIL_GROUP_HEADS)
        o_g, l_g = dilated_group_attention(dqkv[0][:, hs], dqkv[1][:, hs], dqkv[2][:, hs], window, dilation, slopes[hs])
        outs.append(o_g)
        lses.append(l_g)
    alpha = jax.nn.softmax(jnp.stack(lses), axis=0)
    c = jnp.einsum('gbhs,gbhsd->bhsd', alpha.astype(outs[0].dtype), jnp.stack(outs))
    c = c.transpose(0, 2, 1, 3).reshape(B, S, DIL_OUT)
    gates = jax.nn.sigmoid(h_gate).reshape(B, S, N_BRANCHES, D_MODEL)
    merged = (gates[:, :, 0] * jnp.einsum('bsk,kd->bsd', a, w_br_mla)
              + gates[:, :, 1] * jnp.einsum('bsk,kd->bsd', b, w_br_moba)
              + gates[:, :, 2] * jnp.einsum('bsk,kd->bsd', c, w_br_dil))
    return jnp.einsum('bsd,de->bse', merged, w_out)


def setup_inputs(seed: int = 0) -> dict:
    key = jax.random.key(seed)
    ks = jax.random.split(key, 20)

    def nrm(k, shape, fan_in):
        return jax.random.normal(k, shape, jnp.float32) * (fan_in ** -0.5)

    def gain(k, shape):
        return 1.0 + 0.01 * jax.random.normal(k, shape, jnp.float32)

    return {
        'x': jax.random.normal(ks[0], (BATCH, SEQ, D_MODEL), jnp.float32),
        'ffn1_norm': gain(ks[1], (DEPTH, D_MODEL)),
        'ffn1_w_gate': nrm(ks[2], (DEPTH, D_MODEL, D_FF), D_MODEL),
        'ffn1_w_up': nrm(ks[3], (DEPTH, D_MODEL, D_FF), D_MODEL),
        'ffn1_w_down': nrm(ks[4], (DEPTH, D_FF, D_MODEL), D_FF),
        'mix_norm': gain(ks[5], (DEPTH, D_MODEL)),
        'w_in': nrm(ks[6], (DEPTH, D_MODEL, IN_COLS), D_MODEL),
        'q_norm': gain(ks[7], (DEPTH, MLA_Q_RANK)),
        'w_uq': nrm(ks[8], (DEPTH, MLA_Q_RANK, MLA_HEADS * (MLA_NOPE + MLA_ROPE)), MLA_Q_RANK),
        'kv_norm': gain(ks[9], (DEPTH, MLA_KV_RANK)),
        'w_ukv': nrm(ks[10], (DEPTH, MLA_KV_RANK, MLA_HEADS * (MLA_NOPE + MLA_V)), MLA_KV_RANK),
        'w_br_mla': nrm(ks[11], (DEPTH, MLA_OUT, D_MODEL), MLA_OUT),
        'w_br_moba': nrm(ks[12], (DEPTH, MOBA_OUT, D_MODEL), MOBA_OUT),
        'w_br_dil': nrm(ks[13], (DEPTH, DIL_OUT, D_MODEL), DIL_OUT),
        'w_out': nrm(ks[14], (DEPTH, D_MODEL, D_MODEL), D_MODEL),
        'ffn2_norm': gain(ks[15], (DEPTH, D_MODEL)),
        'ffn2_w_gate': nrm(ks[16], (DEPTH, D_MODEL, D_FF), D_MODEL),
        'ffn2_w_up': nrm(ks[17], (DEPTH, D_MODEL, D_FF), D_MODEL),
        'ffn2_w_down': nrm(ks[18], (DEPTH, D_FF, D_MODEL), D_FF),
        'final_norm': gain(ks[19], (D_MODEL,)),
    }


def reference(x, ffn1_norm, ffn1_w_gate, ffn1_w_up, ffn1_w_down, mix_norm, w_in, q_norm, w_uq,
              kv_norm, w_ukv, w_br_mla, w_br_moba, w_br_dil, w_out, ffn2_norm, ffn2_w_gate,
              ffn2_w_up, ffn2_w_down, final_norm):
    for l in range(DEPTH):
        x = x + 0.5 * swiglu(rms_norm(x, ffn1_norm[l]), ffn1_w_gate[l], ffn1_w_up[l], ffn1_w_down[l])
        x = x + hybrid_mixer(rms_norm(x, mix_norm[l]), w_in[l], q_norm[l], w_uq[l], kv_norm[l], w_ukv[l],
                             w_br_mla[l], w_br_moba[l], w_br_dil[l], w_out[l])
        x = x + 0.5 * swiglu(rms_norm(x, ffn2_norm[l]), ffn2_w_gate[l], ffn2_w_up[l], ffn2_w_down[l])
    return rms_norm(x, final_norm)
```

```python
import numpy as np
from contextlib import ExitStack
import concourse.bass as bass
import concourse.mybir as mybir
from concourse.bass_utils import run_bass_kernel_spmd

F32 = mybir.dt.float32
BF16 = mybir.dt.bfloat16
AF = mybir.ActivationFunctionType
ALU = mybir.AluOpType
AX = mybir.AxisListType

D = 1024
S = 4096
DFF = 2816
NL = 4
NCORES = 8
EPS = 1e-6
IN_COLS = 6944
ENGS = ("pe", "act", "dve", "pool", "sp")


class Prog:
    def __init__(self, nc, n_dma_sems=48):
        self.nc = nc
        self.q = {e: [] for e in ENGS}
        self.cnt = {e: 0 for e in ENGS}
        self.seen = {e: {} for e in ENGS}
        self.lastw = {}
        self.reads = {}
        self.n_dma_sems = n_dma_sems
        self.dma_i = 0
        self.dma_i_sw = 0
        self.dma_val = [0] * n_dma_sems
        self.sems = {}
        self.local_dma = []

    def _deps(self, eng, reads, writes):
        need = {}

        def add(tok):
            if tok is None:
                return
            k, v = tok
            if k == "pe" and eng == "pe":
                return
            if need.get(k, 0) < v:
                need[k] = v

        for r in reads:
            add(self.lastw.get(r))
        for w in writes:
            add(self.lastw.get(w))
            for k, v in self.reads.get(w, {}).items():
                add((k, v))
        out = []
        for k, v in need.items():
            if self.seen[eng].get(k, 0) < v:
                self.seen[eng][k] = v
                out.append((k, v))
        return out

    def _commit(self, tok, reads, writes):
        k, v = tok
        for r in reads:
            d = self.reads.setdefault(r, {})
            if d.get(k, 0) < v:
                d[k] = v
        for w in writes:
            self.lastw[w] = tok
            self.reads[w] = {}

    def op(self, eng, fn, reads=(), writes=()):
        waits = self._deps(eng, reads, writes)
        self.cnt[eng] += 1
        tok = (eng, self.cnt[eng])
        self.q[eng].append((waits, fn, (eng, 1)))
        self._commit(tok, reads, writes)
        return tok

    def dma(self, fn, reads=(), writes=(), eng="sp", local=True):
        half = self.n_dma_sems // 2
        if eng == "pool":
            i = half + self.dma_i_sw % half
            self.dma_i_sw += 1
        else:
            i = self.dma_i % half
            self.dma_i += 1
        key = ("dma", i)
        waits = self._deps(eng, reads, writes)
        prev = self.dma_val[i]
        if prev and self.seen[eng].get(key, 0) < prev:
            self.seen[eng][key] = prev
            waits.append((key, prev))
        self.dma_val[i] = prev + 16
        tok = (key, prev + 16)
        self.q[eng].append((waits, fn, (key, 16)))
        self._commit(tok, reads, writes)
        if local:
            self.local_dma.append(tok)
        return tok

    def wait_all(self, eng, toks):
        waits = []
        for k, v in toks:
            if self.seen[eng].get(k, 0) < v:
                self.seen[eng][k] = v
                waits.append((k, v))
        if waits:
            self.q[eng].append((waits, None, None))

    def barrier(self):
        toks = [(e, self.cnt[e]) for e in ("pe", "act", "dve") if self.cnt[e]]
        best = {}
        for k, v in self.local_dma:
            if best.get(k, 0) < v:
                best[k] = v
        toks += list(best.items())
        self.local_dma = []
        for e in ENGS:
            self.wait_all(e, [t for t in toks if t[0] != e])

    def emit(self, ctx):
        nc = self.nc
        keys = list(ENGS) + [("dma", i) for i in range(self.n_dma_sems)]
        for k in keys:
            name = k if isinstance(k, str) else f"dma{k[1]}"
            self.sems[k] = ctx.enter_context(nc.semaphore("s_" + name))
        block = ctx.enter_context(nc.Block())
        sems = self.sems

        def run(e, lst):
            for waits, fn, inc in lst:
                for k, v in waits:
                    e.wait_ge(sems[k], v)
                if fn is not None:
                    fn(e).then_inc(sems[inc[0]], inc[1])

        q = self.q

        @block.tensor
        def _(e):
            run(e, q["pe"])

        @block.scalar
        def _(e):
            run(e, q["act"])

        @block.vector
        def _(e):
            run(e, q["dve"])

        @block.gpsimd
        def _(e):
            run(e, q["pool"])

        @block.sync
        def _(e):
            run(e, q["sp"])


class Arena:
    def __init__(self, nc, ctx, nwords=52800):
        self.t = ctx.enter_context(nc.sbuf_tensor("arena", [128, nwords], F32))
        self.nwords = nwords
        self.top = 0
        self.marks = []

    def alloc(self, shape, dt):
        if isinstance(shape, int):
            shape = (shape,)
        n = int(np.prod(shape))
        esz = 4 if dt == F32 else 2
        words = ((n * esz + 3) // 4 + 7) // 8 * 8
        off = self.top
        self.top += words
        assert self.top <= self.nwords, f"SBUF arena overflow {self.top} > {self.nwords}"
        v = self.t[:, off:off + words]
        if dt != F32:
            v = v.bitcast(dt)
        v = v[:, 0:n]
        if len(shape) == 2:
            v = v.rearrange("p (a b) -> p a b", a=shape[0])
        elif len(shape) == 3:
            v = v.rearrange("p (a b c) -> p a b c", a=shape[0], b=shape[1])
        return v

    def mark(self):
        self.marks.append(self.top)

    def release(self):
        self.top = self.marks.pop()


WSLOT = 3072
NSLOT = 6


class Builder:
    def __init__(self, nlayers=NL, phases=("ffn1", "mix", "ffn2"), debug=False, sub=("mla", "moba", "dil", "merge")):
        self.sub = sub
        self.nlayers = nlayers
        self.phases = phases
        self.debug = debug
        self.nc = nc = bass.Bass("TRN2", target_bir_lowering=False)
        self.ctx = ExitStack()
        dk = "ExternalOutput" if debug else "Internal"
        inp = lambda name, shape: nc.dram_tensor(name, list(shape), F32, kind="ExternalInput").ap()
        self.xT = inp("xT", (D, S))
        self.w = {}
        for nm, shp in [("ffn1_w_gate", (NL, D, DFF)), ("ffn1_w_up", (NL, D, DFF)), ("ffn1_w_down", (NL, DFF, D)),
                        ("ffn2_w_gate", (NL, D, DFF)), ("ffn2_w_up", (NL, D, DFF)), ("ffn2_w_down", (NL, DFF, D)),
                        ("w_in", (NL, D, IN_COLS)), ("w_uq", (NL, 256, 576)), ("w_ukv", (NL, 128, 768)),
                        ("w_br_mla", (NL, 384, D)), ("w_br_moba", (NL, 384, D)), ("w_br_dil", (NL, 256, D)),
                        ("w_out", (NL, D, D))]:
            self.w[nm] = inp(nm, shp)
        self.gains_d = inp("gains", (128, 104 + 8 + 12))
        self.c_rope = inp("c_rope", (64, S))
        self.c_qc = inp("c_qc", (18, 8, S))
        self.c_kc = inp("c_kc", (18, 8, S))
        self.c_onehot = inp("c_onehot", (16, S))
        self.c_cmask = inp("c_cmask", (128, 896))
        self.c_dmask = inp("c_dmask", (128, 2, 512))
        self.c_ident = inp("c_ident", (128, 128))
        self.c_selc = inp("c_selc", (128, 3, 512))
        self.hs = nc.dram_tensor("hs", [D, S], BF16, kind=dk).ap()
        self.brs = nc.dram_tensor("brs", [D, S], BF16, kind=dk).ap()
        self.outT = nc.dram_tensor("outT", [D, S], F32, kind="ExternalOutput").ap()
        self.xs = nc.dram_tensor("xs", [D, S], F32, kind=dk).ap()

    def mm(self, out, lhsT, rhs, start, stop, reads, writes, skip=False):
        if skip:
            self.P.op("pe", lambda e: e.matmul(out, lhsT=lhsT, rhs=rhs, start=start, stop=stop, skip_group_check=True), reads, writes)
        else:
            self.P.op("pe", lambda e: e.matmul(out, lhsT=lhsT, rhs=rhs, start=start, stop=stop), reads, writes)

    def act(self, out, in_, func, reads, writes, bias=None, scale=1.0):
        if bias is None:
            self.P.op("act", lambda e: e.activation(out=out, in_=in_, func=func, scale=scale), reads, writes)
        else:
            self.P.op("act", lambda e: e.activation(out=out, in_=in_, func=func, bias=bias, scale=scale), reads, writes)

    def stt(self, out, in0, scalar, in1, op0, op1, reads, writes, eng="dve"):
        self.P.op(eng, lambda e: e.scalar_tensor_tensor(out=out, in0=in0, scalar=scalar, in1=in1, op0=op0, op1=op1), reads, writes)

    def ts(self, out, in0, s1, s2, op0, op1, reads, writes, eng="dve"):
        if s2 is None:
            self.P.op(eng, lambda e: e.tensor_scalar(out=out, in0=in0, scalar1=s1, scalar2=None, op0=op0), reads, writes)
        else:
            self.P.op(eng, lambda e: e.tensor_scalar(out=out, in0=in0, scalar1=s1, scalar2=s2, op0=op0, op1=op1), reads, writes)

    def rsqrt(self, out, in_, reads, writes):
        self.act(out, in_, AF.Sqrt, list(reads) + ["epsc"], list(writes), bias=self.epsc[:, 0:1])
        self.P.op("dve", lambda e: e.reciprocal(out=out, in_=out), list(writes), list(writes))

    def tt(self, out, in0, in1, op, reads, writes, eng="dve"):
        self.P.op(eng, lambda e: e.tensor_tensor(out=out, in0=in0, in1=in1, op=op), reads, writes)

    def cp(self, out, in_, reads, writes, eng="dve"):
        self.P.op(eng, lambda e: e.tensor_copy(out=out, in_=in_), reads, writes)

    def memset(self, ap, val, writes, eng="dve"):
        self.P.op(eng, lambda e: e.memset(ap, val), (), writes)

    def ld(self, out, in_, reads, writes, eng="sp", local=True):
        return self.P.dma(lambda e: e.dma_start(out=out, in_=in_), reads, writes, eng=eng, local=local)

    def wload(self, parts):
        s = self.w_i % NSLOT
        self.w_i += 1
        slot = self.wring[s]
        res = ("wring", s)
        for k, (dstf, src) in enumerate(parts):
            dst = dstf(slot)
            self.P.dma(lambda e, dst=dst, src=src: e.dma_start(out=dst, in_=src), (), [res] if k == 0 else [(res, k)],
                       eng="pool", local=False)
        allres = [res] + [(res, k) for k in range(1, len(parts))]
        return slot, allres

    def psum(self):
        i = 2 + self.ps_i % 6
        self.ps_i += 1
        return self.psb[i][:], ("ps", i)

    def psum_acc(self):
        i = self.pa_i % 2
        self.pa_i += 1
        return self.psb[i][:], ("ps", i)

    def build(self):
        nc, ctx = self.nc, self.ctx
        self.P = P = Prog(nc)
        self.A = A = Arena(nc, ctx)
        self.psb = [ctx.enter_context(nc.psum_tensor(f"psb{i}", [128, 512], F32)) for i in range(8)]
        self.ps_i = 0
        self.pa_i = 0
        self.pt_i = 0
        self.fin_i = 0
        self.w_i = 0
        self.wring = [A.alloc(WSLOT, BF16) for _ in range(NSLOT)]
        self.gains = A.alloc(124, F32)
        self.ones = {n: A.alloc(128, BF16) for n in (1024, 256, 128)}
        self.ld(self.gains, self.gains_d, (), ["gains"])
        self.epsc = A.alloc(8, F32)
        self.memset(self.epsc, EPS, ["epsc"])
        for n, t in self.ones.items():
            self.memset(t, 1.0 / n, [("ones", n)])
        self.ident = A.alloc(128, BF16)
        self.cmask = A.alloc(896, BF16)
        self.dmask = A.alloc((2, 512), BF16)
        self.wkr = A.alloc((8, 2, 96), BF16)
        self.wqB = A.alloc((2, 6, 96), BF16)
        self.ld(self.ident, self.c_ident, (), ["consts"], eng="pool")
        self.ld(self.cmask, self.c_cmask, (), [("consts", 1)], eng="pool")
        self.ld(self.dmask, self.c_dmask, (), [("consts", 2)], eng="pool")
        self.memset(self.wkr, 0.0, ["wkr"])
        self.memset(self.wqB, 0.0, ["wqB"])
        self.deferred = []
        self.final_toks = []
        self.no_i = 0
        P.barrier()
        if self.phases[0] == "ffn1":
            self.pre_norm(0, 0)
        else:
            for i in range(8):
                self.ld(self.xs[i * 128:(i + 1) * 128, :], self.xT[i * 128:(i + 1) * 128, :], (), [("xs", tg) for tg in range(4)])
            self.pre_norm(32, 0)
        P.barrier()
        seq = [(l, ph) for l in range(self.nlayers) for ph in ("ffn1", "mix", "ffn2") if ph in self.phases]
        gb = {"ffn1": 0, "mix": 32, "ffn2": 72}
        for i, (l, ph) in enumerate(seq):
            nxt = (gb[seq[i + 1][1]], seq[i + 1][0]) if i + 1 < len(seq) else None
            if ph == "mix":
                self.mixer(l, nxt)
            else:
                self.ffn(l, ph, gb[ph], nxt)
            P.barrier()
        P.wait_all("sp", self.final_toks)
        with nc.allow_low_precision(reason="bf16 matmul operands, fp32 accumulation (reference tolerance is bf16-level)"):
            P.emit(ctx)
        ctx.close()
        return nc

    def pre_norm(self, gbase, l):
        A = self.A
        xT_v = self.xT.rearrange("(c p) t -> p c t", p=128)
        A.mark()
        xg = [A.alloc((8, 512), F32) for _ in range(2)]
        self.alloc_norm_bufs(False)
        for t in range(8):
            b = t % 2
            self.ld(xg[b], xT_v[:, :, t * 512:(t + 1) * 512], (), [("px", b)])
            self.norm_out(xg[b], ("px", b), t, gbase, l)
        A.release()

    def gcol(self, base, l, c):
        k = base + l * 8 + c
        return self.gains[:, k:k + 1]

    def norm_tile(self, X, xres, hdst, hres, gbase, l, sq, rstd, n=512):
        self.act(sq[:, :, 0:n], X, AF.Square, [xres], ["sq"])
        ps, pr = self.psum()
        for c in range(8):
            self.mm(ps[:, 0:n], self.ones[1024], sq[:, c, 0:n], c == 0, c == 7, ["sq", ("ones", 1024)], [pr])
        self.rsqrt(rstd[:, 0:n], ps[:, 0:n], [pr], ["rstd"])
        for c in range(8):
            self.stt(hdst[:, c, :], X[:, c, :], self.gcol(gbase, l, c), rstd[:, 0:n], ALU.mult, ALU.mult,
                     [xres, "rstd", "gains"], [hres])

    def alloc_norm_bufs(self, final=False):
        A = self.A
        self.no_sq = A.alloc((8, 512), BF16)
        self.no_rstd = A.alloc(512, F32)
        self.no_final = final
        if final:
            self.no_og = A.alloc((8, 512), F32)
        else:
            self.no_hb = [A.alloc((8, 512), BF16) for _ in range(2)]

    def norm_out(self, X, xres, t_idx, gbase, l):
        gsl = slice(t_idx * 512, (t_idx + 1) * 512)
        final = self.no_final
        i = self.no_i % 2
        self.no_i += 1
        dst, dres = (self.no_og, "no_og") if final else (self.no_hb[i], ("no_hb", i))
        self.act(self.no_sq, X, AF.Square, [xres], ["no_sq"])
        ps, pr = self.psum()
        for c in range(8):
            self.mm(ps, self.ones[1024], self.no_sq[:, c, :], c == 0, c == 7, ["no_sq", ("ones", 1024)], [pr])
        self.rsqrt(self.no_rstd, ps, [pr], ["no_rstd"])
        for c in range(8):
            k = 104 + c if final else gbase + l * 8 + c
            self.stt(dst[:, c, :], X[:, c, :], self.gains[:, k:k + 1], self.no_rstd, ALU.mult, ALU.mult,
                     [xres, "no_rstd", "gains"], [dres])
        if final:
            out_v = self.outT.rearrange("(c p) t -> p c t", p=128)
            self.final_toks.append(self.ld(out_v[:, :, gsl], dst, [dres], [("out", t_idx)]))
        else:
            hs_v = self.hs.rearrange("(c p) t -> p c t", p=128)
            self.ld(hs_v[:, :, gsl], dst, [dres], [("hs", t_idx)])

    def flush_deferred(self):
        for f in self.deferred:
            f()
        self.deferred = []

    def ffn(self, l, name, gbase, nxt):
        P, A = self.P, self.A
        wg = self.w[name + "_w_gate"][l].rearrange("(c p) f -> p c f", p=128)
        wu = self.w[name + "_w_up"][l].rearrange("(c p) f -> p c f", p=128)
        wd = self.w[name + "_w_down"][l].rearrange("(c p) d -> p c d", p=128)
        xs_v = self.xs.rearrange("(c p) t -> p c t", p=128)
        hs_v = self.hs.rearrange("(c p) t -> p c t", p=128)
        src_v = self.xT.rearrange("(c p) t -> p c t", p=128) if (l == 0 and name == "ffn1") else xs_v
        A.mark()
        TG = 1024
        xg = [A.alloc((8, TG), F32) for _ in range(2)]
        hT = A.alloc((8, TG), BF16)
        actT = A.alloc((22, TG), BF16)
        sg = [A.alloc(512, BF16) for _ in range(2)]
        self.alloc_norm_bufs(final=(nxt is None))
        def load_group(tg):
            sl_ = slice(tg * TG, (tg + 1) * TG)
            self.ld(xg[tg % 2], src_v[:, :, sl_], [("xs", tg)], [("xg", tg % 2, 0), ("xg", tg % 2, 1)])
            self.ld(hT, hs_v[:, :, sl_], [("hs", 2 * tg), ("hs", 2 * tg + 1)], [("hT", 0), ("hT", 1)])

        load_group(0)
        for tg in range(S // TG):
            b = tg % 2
            X = xg[b]
            sl = slice(tg * TG, (tg + 1) * TG)
            k = 0
            for fp in range(11):
                if fp == 2:
                    self.flush_deferred()
                fs = slice(fp * 256, (fp + 1) * 256)
                g16, gres = self.wload([(lambda s: s.rearrange("p (c f) -> p c f", c=8)[:, :, 0:256], wg[:, :, fs])])
                u16, ures = self.wload([(lambda s: s.rearrange("p (c f) -> p c f", c=8)[:, :, 0:256], wu[:, :, fs])])
                g16 = g16.rearrange("p (c f) -> p c f", c=8)
                u16 = u16.rearrange("p (c f) -> p c f", c=8)
                for half in range(2):
                    fc = fp * 2 + half
                    hs = slice(half * 128, (half + 1) * 128)
                    for st in range(2):
                        ts_ = slice(st * 512, (st + 1) * 512)
                        pg, pgr = self.psum()
                        for c in range(8):
                            self.mm(pg, g16[:, c, hs], hT[:, c, ts_], c == 0, c == 7, gres + [("hT", st)], [pgr])
                        pu, pur = self.psum()
                        for c in range(8):
                            self.mm(pu, u16[:, c, hs], hT[:, c, ts_], c == 0, c == 7, ures + [("hT", st)], [pur])
                        s_ = sg[k % 2]
                        self.act(s_, pg, AF.Silu, [pgr], [("sg", k % 2)])
                        self.tt(actT[:, fc, ts_], s_, pu, ALU.mult, [("sg", k % 2), pur], [("actT", fc, st)])
                        k += 1
            if tg + 1 < S // TG:
                load_group(tg + 1)
            for dc in range(8):
                ds_ = slice(dc * 128, (dc + 1) * 128)
                d16, dres = self.wload([(lambda s: s.rearrange("p (c f) -> p c f", c=24)[:, 0:22, :], wd[:, :, ds_])])
                d16 = d16.rearrange("p (c f) -> p c f", c=24)
                for st in range(2):
                    ts_ = slice(st * 512, (st + 1) * 512)
                    po, por = self.psum()
                    for fc in range(22):
                        self.mm(po, d16[:, fc, :], actT[:, fc, ts_], fc == 0, fc == 21, dres + [("actT", fc, st)], [por])
                    self.stt(X[:, dc, ts_], po, 0.5, X[:, dc, ts_], ALU.mult, ALU.add, [por, ("xg", b, st)], [("xg", b, st)])
            if nxt is not None:
                self.ld(xs_v[:, :, sl], X, [("xg", b, 0), ("xg", b, 1)], [("xs", tg)])
            ngb, nl = nxt if nxt is not None else (0, 0)
            for st in range(2):
                self.deferred.append(lambda X=X, b=b, st=st, tg=tg, ngb=ngb, nl=nl: self.norm_out(
                    X[:, :, st * 512:(st + 1) * 512], ("xg", b, st), 2 * tg + st, ngb, nl))
        self.flush_deferred()
        A.release()

    def load_hT(self, hT):
        hs_v = self.hs.rearrange("(c p) t -> p c t", p=128)
        for t in range(8):
            sl = slice(t * 512, (t + 1) * 512)
            self.ld(hT[:, :, sl], hs_v[:, :, sl], [("hs", t)], [("hT", t)])

    def attn_finish(self, acc_ap, accres, row0, qs, qt):
        i = self.fin_i % 2
        self.fin_i += 1
        osb, rd, ob = self.osb[i], self.rd[i], self.ob[i]
        self.cp(osb, acc_ap, [accres], [("osb", i)])
        self.P.op("dve", lambda e: e.reciprocal(out=rd[64:128, :], in_=osb[64:128, :]), [("osb", i)], [("rd", i)])
        pr, prr = self.psum()
        self.mm(pr[0:64, :], self.ident[64:128, 64:128], rd[64:128, :], True, True, [("rd", i), "consts"], [prr])
        self.tt(ob[0:64, :], osb[0:64, :], pr[0:64, :], ALU.mult, [("osb", i), prr], [("ob", i)])
        self.ld(self.brs[row0:row0 + 64, qs], ob[0:64, :], [("ob", i)], [("brs", row0, qt)])

    def attn_causal(self, Qa, Ka, Va, qres, kres, vres, KD, scale, row0):
        LA = 2
        pairs = [(qt, kt) for qt in range(8) for kt in range(4 * qt + 4)]
        st = {}
        pos = {}
        for i in range(len(pairs) + LA):
            if i < len(pairs):
                qt, kt = pairs[i]
                qs = slice(qt * 512, (qt + 1) * 512)
                ps, psr = self.psum()
                diag = kt >= 4 * qt
                if diag:
                    jj = kt - 4 * qt
                    c0 = 384 - 128 * jj
                    self.mm(ps, self.ident, self.cmask[:, c0:c0 + 512], True, False, ["consts", ("consts", 1)], [psr])
                self.mm(ps, Ka[0:KD, kt * 128:(kt + 1) * 128], Qa[0:KD, qs], not diag, True, kres(kt) + qres(qt), [psr])
                pi = self.pt_i % 4
                self.pt_i += 1
                self.act(self.pT[pi], ps, AF.Exp, [psr], [("pT", pi)], scale=scale)
                st[i] = pi
            j = i - LA
            if j >= 0:
                qt, kt = pairs[j]
                nk = 4 * qt + 4
                if kt == 0:
                    pos[qt] = self.psum_acc()
                po, por = pos[qt]
                pi = st.pop(j)
                self.mm(po, Va[:, kt, :], self.pT[pi], kt == 0, kt == nk - 1, vres(kt) + [("pT", pi)], [por])
                if kt == nk - 1:
                    self.attn_finish(po, por, row0, slice(qt * 512, (qt + 1) * 512), qt)
            yield

    def alloc_attn_bufs(self):
        A = self.A
        self.Qa = [A.alloc(S, BF16) for _ in range(2)]
        self.Ka = [A.alloc(S, BF16) for _ in range(2)]
        self.Va = [A.alloc((32, 128), BF16) for _ in range(2)]
        self.pT = [A.alloc(512, BF16) for _ in range(4)]
        self.osb = [A.alloc(512, F32) for _ in range(2)]
        self.rd = [A.alloc(512, BF16) for _ in range(2)]
        self.ob = [A.alloc(512, BF16) for _ in range(2)]
        for b in range(2):
            self.memset(self.Va[b][:, :, 64:128], 1.0, [("Va1", b)])
            self.memset(self.Qa[b], 0.0, [("Qa0", b)])
            self.memset(self.Ka[b], 0.0, [("Ka0", b)])

    def proj_qkv(self, w16, wres, hT, Qa_b, Ka_b, Va_b, vT, b, tokfn):
        for t in range(8):
            ts_ = slice(t * 512, (t + 1) * 512)
            ps, psr = self.psum()
            for c in range(8):
                self.mm(ps[0:64, :], w16[:, c, 0:64], hT[:, c, ts_], c == 0, c == 7, wres + [("hT", t)], [psr])
            self.act(Qa_b[0:64, ts_], ps[0:64, :], AF.Copy, [psr, ("Qa0", b)], [("Qa", b, "q", t)])
            yield
            ps, psr = self.psum()
            for c in range(8):
                self.mm(ps, w16[:, c, 64:192], hT[:, c, ts_], c == 0, c == 7, wres + [("hT", t)], [psr])
            self.act(Ka_b[0:64, ts_], ps[0:64, :], AF.Copy, [psr, ("Ka0", b)], [("Ka", b, "q", t)])
            self.cp(vT[64:128, ts_], ps[64:128, :], [psr], [("vT", t)])
            yield
        vres = [("vT", t) for t in range(8)] + ["consts"]
        for t8 in range(4):
            ps, psr = self.psum()
            for j in range(8):
                ti = t8 * 8 + j
                self.mm(ps[:, j * 64:(j + 1) * 64], vT[64:128, tokfn(ti)], self.ident[64:128, 64:128], j == 0, True, vres, [psr], skip=True)
            self.cp(Va_b[:, t8 * 8:(t8 + 1) * 8, 0:64], ps.rearrange("p (a b) -> p a b", a=8), [psr, ("Va1", b)], [("Va", b, t8)])
            yield

    def mixer(self, l, nxt):
        P, A = self.P, self.A
        win = self.w["w_in"][l].rearrange("(c p) k -> p c k", p=128)
        xs_v = self.xs.rearrange("(c p) t -> p c t", p=128)
        hs_v = self.hs.rearrange("(c p) t -> p c t", p=128)
        v192 = lambda s: s.rearrange("p (c f) -> p c f", c=8)[:, :, 0:192]

        if "mla" in self.sub:
            self.mla(l, win)
        if "moba" in self.sub:
            self.moba(l, win, v192)
        if "dil" in self.sub:
            self.dil(l, win, v192)
        if "merge" in self.sub:
            self.merge(l, win, nxt)

    def mla(self, l, win):
        P, A = self.P, self.A
        A.mark()
        cqn = A.alloc((2, S), BF16)
        ckvn = A.alloc(S, BF16)
        krot = A.alloc(S, BF16)
        cosT = A.alloc(S, BF16)
        sinT = A.alloc(S, BF16)
        self.ld(cosT[64:96, :], self.c_rope[0:32, :], (), ["cosT"], eng="pool")
        self.ld(sinT[64:96, :], self.c_rope[32:64, :], (), ["sinT"], eng="pool")
        A.mark()
        hT = A.alloc((8, S), BF16)
        tmp32 = A.alloc((3, 512), F32)
        sq3 = A.alloc((3, 512), BF16)
        rstd2 = A.alloc((2, 512), F32)
        t1 = A.alloc(512, F32)
        t2 = A.alloc(512, F32)
        self.load_hT(hT)
        wlat, wres = self.wload([(lambda s: s.rearrange("p (c f) -> p c f", c=8), win[:, :, 0:384])])
        wlat = wlat.rearrange("p (c f) -> p c f", c=8)
        self.ld(self.wkr[:, :, 0, 64:96], win[:, :, 384:416], (), ["wkr"], eng="pool")
        self.ld(self.wkr[:, :, 1, 64:80], win[:, :, 400:416], (), [("wkr", 1)], eng="pool")
        self.ld(self.wkr[:, :, 1, 80:96], win[:, :, 384:400], (), [("wkr", 2)], eng="pool")
        wkres = ["wkr", ("wkr", 1), ("wkr", 2)]
        for t in range(8):
            ts_ = slice(t * 512, (t + 1) * 512)
            for j in range(3):
                ps, psr = self.psum()
                for c in range(8):
                    self.mm(ps, wlat[:, c, j * 128:(j + 1) * 128], hT[:, c, ts_], c == 0, c == 7, wres + [("hT", t)], [psr])
                self.act(tmp32[:, j, :], ps, AF.Copy, [psr], [("tmp32", j)])
            self.act(sq3, tmp32, AF.Square, [("tmp32", j) for j in range(3)], ["sq3"])
            ps, psr = self.psum()
            for j in range(2):
                self.mm(ps, self.ones[256], sq3[:, j, :], j == 0, j == 1, ["sq3", ("ones", 256)], [psr])
            self.rsqrt(rstd2[:, 0, :], ps, [psr], [("rstd2", 0)])
            ps, psr = self.psum()
            self.mm(ps, self.ones[128], sq3[:, 2, :], True, True, ["sq3", ("ones", 128)], [psr])
            self.rsqrt(rstd2[:, 1, :], ps, [psr], [("rstd2", 1)])
            for j in range(2):
                k = 64 + l * 2 + j
                self.stt(cqn[:, j, ts_], tmp32[:, j, :], self.gains[:, k:k + 1], rstd2[:, 0, :], ALU.mult, ALU.mult,
                         [("tmp32", j), ("rstd2", 0), "gains"], [("cqn", t)])
            k = 112 + l
            self.stt(ckvn[:, ts_], tmp32[:, 2, :], self.gains[:, k:k + 1], rstd2[:, 1, :], ALU.mult, ALU.mult,
                     [("tmp32", 2), ("rstd2", 1), "gains"], [("ckvn", t)])
            pa, par = self.psum()
            for c in range(8):
                self.mm(pa[0:96, :], self.wkr[:, c, 0, :], hT[:, c, ts_], c == 0, c == 7, wkres + [("hT", t)], [par])
            pb, pbr = self.psum()
            for c in range(8):
                self.mm(pb[0:96, :], self.wkr[:, c, 1, :], hT[:, c, ts_], c == 0, c == 7, wkres + [("hT", t)], [pbr])
            self.tt(t1[64:96, :], pa[64:96, :], cosT[64:96, ts_], ALU.mult, [par, "cosT"], ["t1"])
            self.tt(t2[64:96, :], pb[64:96, :], sinT[64:96, ts_], ALU.mult, [pbr, "sinT"], ["t2"])
            self.tt(krot[64:96, ts_], t1[64:96, :], t2[64:96, :], ALU.add, ["t1", "t2"], ["krot"])
        A.release()
        P.barrier()
        A.mark()
        self.alloc_attn_bufs()
        t1 = A.alloc(512, F32)
        t2 = A.alloc(512, F32)
        wuq_d = self.w["w_uq"][l].rearrange("(c p) k -> p c k", p=128)
        wuq, wuqres = self.wload([(lambda s: s[:, 0:1152].rearrange("p (c f) -> p c f", c=2), wuq_d)])
        wuq = wuq[:, 0:1152].rearrange("p (c f) -> p c f", c=2)
        wukv, wukvres = self.wload([(lambda s: s[:, 0:768], self.w["w_ukv"][l])])
        wqres = []
        for h in range(6):
            self.ld(self.wqB[:, :, h, 64:80], wuq_d[:, :, h * 96 + 80:h * 96 + 96], (), [("wqB", h, 0)], eng="pool")
            self.ld(self.wqB[:, :, h, 80:96], wuq_d[:, :, h * 96 + 64:h * 96 + 80], (), [("wqB", h, 1)], eng="pool")
        self.run_heads(6, lambda h: self.mla_prep(h, cqn, ckvn, krot, cosT, sinT, t1, t2, wuq, wuqres, wukv, wukvres),
                       lambda h: self.attn_causal(self.Qa[h % 2], self.Ka[h % 2], self.Va[h % 2],
                                                  lambda qt, b=h % 2: [("Qa", b, "q", qt), ("Qa", b, "r", qt)],
                                                  lambda kt, b=h % 2: [("Ka", b, "q", kt // 4), ("Ka", b, "r")],
                                                  lambda kt, b=h % 2: [("Va", b, kt // 8)],
                                                  96, 96.0 ** -0.5, h * 64), every=3)
        A.release()
        A.release()
        P.barrier()

    def mla_prep(self, h, cqn, ckvn, krot, cosT, sinT, t1, t2, wuq, wuqres, wukv, wukvres):
        if True:
            b = h % 2
            Qa, Ka, Va = self.Qa[b], self.Ka[b], self.Va[b]
            wqr = ["wqB", ("wqB", h, 0), ("wqB", h, 1)]
            for t in range(8):
                ts_ = slice(t * 512, (t + 1) * 512)
                p1, p1r = self.psum()
                for c in range(2):
                    self.mm(p1[0:96, :], wuq[:, c, h * 96:(h + 1) * 96], cqn[:, c, ts_], c == 0, c == 1, wuqres + [("cqn", t)], [p1r])
                p2, p2r = self.psum()
                for c in range(2):
                    self.mm(p2[0:96, :], self.wqB[:, c, h, :], cqn[:, c, ts_], c == 0, c == 1, wqr + [("cqn", t)], [p2r])
                self.act(Qa[0:64, ts_], p1[0:64, :], AF.Copy, [p1r, ("Qa0", b)], [("Qa", b, "q", t)])
                self.tt(t1[64:96, :], p1[64:96, :], cosT[64:96, ts_], ALU.mult, [p1r, "cosT"], ["t1"])
                self.tt(t2[64:96, :], p2[64:96, :], sinT[64:96, ts_], ALU.mult, [p2r, "sinT"], ["t2"])
                self.tt(Qa[64:96, ts_], t1[64:96, :], t2[64:96, :], ALU.add, ["t1", "t2", ("Qa0", b)], [("Qa", b, "r", t)])
                pk, pkr = self.psum()
                self.mm(pk[0:64, :], wukv[:, h * 128:h * 128 + 64], ckvn[:, ts_], True, True, wukvres + [("ckvn", t)], [pkr])
                self.act(Ka[0:64, ts_], pk[0:64, :], AF.Copy, [pkr, ("Ka0", b)], [("Ka", b, "q", t)])
                yield
            self.cp(Ka[64:96, :], krot[64:96, :], ["krot", ("Ka0", b)], [("Ka", b, "r")])
            for t8 in range(4):
                ps, psr = self.psum()
                for j in range(8):
                    kt = t8 * 8 + j
                    self.mm(ps[:, j * 64:(j + 1) * 64], ckvn[:, kt * 128:(kt + 1) * 128], wukv[:, h * 128 + 64:h * 128 + 128],
                            j == 0, True, wukvres + [("ckvn", kt // 4)], [psr], skip=True)
                self.cp(Va[:, t8 * 8:(t8 + 1) * 8, 0:64], ps.rearrange("p (a b) -> p a b", a=8), [psr, ("Va1", b)], [("Va", b, t8)])
                yield

    def moba(self, l, win, v192):
        P, A = self.P, self.A
        A.mark()
        hT = A.alloc((8, S), BF16)
        self.load_hT(hT)
        self.alloc_attn_bufs()
        km32 = A.alloc(16, F32)
        km16 = A.alloc(16, BF16)
        g0 = A.alloc(512, F32)
        g1 = A.alloc(512, F32)
        g2 = A.alloc(512, F32)
        eq = A.alloc(512, F32)
        mx = A.alloc(32, F32)
        selc = A.alloc((3, 512), F32)
        stgb = A.alloc((32, 80), BF16)
        vT = A.alloc(S, BF16)
        self.memset(stgb, 0.0, ["stgb"])
        self.ld(selc, self.c_selc, (), ["selc"])
        base = 416
        self.run_heads(6, lambda h: self.moba_prep(h, hT, win, v192, base, (km32, km16, g0, g1, g2, eq, mx, selc, stgb, vT)),
                       lambda h: self.moba_attend(h))
        A.release()
        P.barrier()

    def run_heads(self, n, prep, attend, every=2):
        for _ in prep(0):
            pass
        for h in range(n):
            nxt = prep(h + 1) if h + 1 < n else None
            for k, _ in enumerate(attend(h)):
                if nxt is not None and k % every == every - 1:
                    next(nxt, None)
            if nxt is not None:
                for _ in nxt:
                    pass

    def moba_attend(self, h):
        b = h % 2
        return self.attn_causal(self.Qa[b], self.Ka[b], self.Va[b],
                                lambda qt, b=b: [("Qa", b, "q", qt), ("Qa", b, "c"), ("Qa", b, "s", qt)],
                                lambda kt, b=b: [("Ka", b, "q", kt // 4), ("Ka", b, "c"), ("Ka", b, "c2")],
                                lambda kt, b=b: [("Va", b, kt // 8)],
                                88, 0.125, 384 + h * 64)

    def moba_prep(self, h, hT, win, v192, base, bufs):
        km32, km16, g0, g1, g2, eq, mx, selc, stgb, vT = bufs
        if True:
            b = h % 2
            Qa, Ka, Va = self.Qa[b], self.Ka[b], self.Va[b]
            w16, wres = self.wload([(lambda s: v192(s)[:, :, 0:64], win[:, :, base + h * 64:base + h * 64 + 64]),
                                    (lambda s: v192(s)[:, :, 64:128], win[:, :, base + 384 + h * 64:base + 384 + h * 64 + 64]),
                                    (lambda s: v192(s)[:, :, 128:192], win[:, :, base + 768 + h * 64:base + 768 + h * 64 + 64])])
            w16 = v192(w16)
            self.ld(Qa[80:88, :], self.c_qc[12 + h], [("Qa0", b)], [("Qa", b, "c")], eng="pool")
            self.ld(Ka[64:80, :], self.c_onehot, [("Ka0", b)], [("Ka", b, "c")], eng="pool")
            self.ld(Ka[80:88, :], self.c_kc[12 + h], [("Ka0", b)], [("Ka", b, "c2")], eng="pool")
            yield
            yield from self.proj_qkv(w16, wres, hT, Qa, Ka, Va, vT, b, lambda ti: slice(ti * 128, (ti + 1) * 128))
            self.P.op("dve", lambda e, Ka=Ka: e.tensor_reduce(out=km32[0:64, :], in_=Ka[0:64, :].rearrange("p (n k) -> p n k", k=256),
                                                             axis=AX.X, op=ALU.add),
                      [("Ka", b, "q", t) for t in range(8)], ["km32"])
            self.ts(km16[0:64, :], km32[0:64, :], 1.0 / 256.0, None, ALU.mult, None, ["km32"], ["km16"])
            yield
            pg, pgr = self.psum()
            for qt in range(32):
                self.mm(pg[:, qt * 16:(qt + 1) * 16], Qa[0:64, qt * 128:(qt + 1) * 128], km16[0:64, :], qt == 0, True,
                        [("Qa", b, "q", qt // 4), "km16"], [pgr], skip=True)
            v3 = lambda ap: ap.rearrange("p (a n) -> p a n", n=16)
            bc = lambda ap: ap.unsqueeze(2).to_broadcast([128, 32, 16])
            self.tt(g0, pg, selc[:, 0, :], ALU.mult, [pgr, "selc"], ["g0"])
            yield
            self.tt(g0, g0, selc[:, 1, :], ALU.add, ["g0", "selc"], ["g0"])
            src = g0
            for rnd, gdst in enumerate((g1, g2)):
                self.P.op("dve", lambda e, src=src: e.tensor_reduce(out=mx, in_=v3(src), axis=AX.X, op=ALU.max), ["g0", "g1", "g2"], ["mx"])
                self.tt(v3(eq), v3(src), bc(mx), ALU.is_ge, ["mx", "g0", "g1", "g2"], ["eq"])
                self.stt(gdst, eq, -2e30, src, ALU.mult, ALU.add, ["eq", "g0", "g1", "g2"], ["g1", "g2"])
                src = gdst
            self.P.op("dve", lambda e: e.tensor_reduce(out=mx, in_=v3(g2), axis=AX.X, op=ALU.max), ["g0", "g1", "g2"], ["mx"])
            self.tt(v3(eq), v3(g0), bc(mx), ALU.is_ge, ["mx", "g0"], ["eq"])
            self.ts(eq, eq, 1.0, 30000.0, ALU.subtract, ALU.mult, ["eq"], ["eq"])
            self.tt(stgb[:, :, 64:80], v3(eq), v3(selc[:, 2, :]), ALU.mult, ["eq", "selc"], ["stgb"])
            for q4 in range(8):
                pt, ptr = self.psum()
                for j in range(4):
                    qt = q4 * 4 + j
                    self.mm(pt[0:80, j * 128:(j + 1) * 128], stgb[:, qt, :], self.ident, j == 0, True, ["stgb", "consts"], [ptr], skip=True)
                self.act(Qa[64:80, q4 * 512:(q4 + 1) * 512], pt[64:80, :], AF.Copy, [ptr, ("Qa0", b)], [("Qa", b, "s", q4)])
                yield

    def dil(self, l, win, v192):
        P, A = self.P, self.A
        A.mark()
        hT = A.alloc((8, S), BF16)
        self.load_hT(hT)
        self.alloc_attn_bufs()
        acc = A.alloc(S, F32)
        vT = A.alloc(S, BF16)
        base = 1568
        idx = 0
        for j in range(4):
            for g in range(3):
                dl = (1, 4, 16)[g]
                nbk = 32 // dl
                hd = 4 * g + j
                b = idx % 2
                idx += 1
                Qa, Ka, Va = self.Qa[b], self.Ka[b], self.Va[b]
                w16, wres = self.wload([(lambda s: v192(s)[:, :, 0:64], win[:, :, base + hd * 64:base + hd * 64 + 64]),
                                        (lambda s: v192(s)[:, :, 64:128], win[:, :, base + 768 + hd * 64:base + 768 + hd * 64 + 64]),
                                        (lambda s: v192(s)[:, :, 128:192], win[:, :, base + 1536 + hd * 64:base + 1536 + hd * 64 + 64])])
                w16 = v192(w16)
                self.ld(Qa[64:72, :], self.c_qc[hd], [("Qa0", b)], [("Qa", b, "c")], eng="pool")
                self.ld(Ka[64:72, :], self.c_kc[hd], [("Ka0", b)], [("Ka", b, "c")], eng="pool")

                def tok(ti, dl=dl, nbk=nbk):
                    r, nb = divmod(ti, nbk)
                    s0 = r + dl * 128 * nb
                    return slice(s0, s0 + dl * 127 + 1, dl)

                for _ in self.proj_qkv(w16, wres, hT, Qa, Ka, Va, vT, b, tok):
                    pass
                qall = [("Qa", b, "q", t) for t in range(8)] + [("Qa", b, "c")]
                kall = [("Ka", b, "q", t) for t in range(8)] + [("Ka", b, "c")]
                stA = {}
                for gi in range(9):
                    if gi < 8:
                        grp = gi
                        pown, pownr = self.psum()
                        pprev, pprevr = self.psum()
                        self.mm(pown, self.ident, self.dmask[:, 0, :], True, False, ["consts", ("consts", 2)], [pownr])
                        self.mm(pprev, self.ident, self.dmask[:, 1, :], True, False, ["consts", ("consts", 2)], [pprevr])
                        for qb in range(4):
                            ti = grp * 4 + qb
                            nb = ti % nbk
                            cs = slice(qb * 128, (qb + 1) * 128)
                            self.mm(pown[:, cs], Ka[0:72, tok(ti)], Qa[0:72, tok(ti)], False, True, qall + kall, [pownr], skip=True)
                            tp = ti - 1 if nb > 0 else ti
                            self.mm(pprev[:, cs], Ka[0:72, tok(tp)], Qa[0:72, tok(ti)], False, True, qall + kall, [pprevr], skip=True)
                        i0 = self.pt_i % 4
                        i1 = (self.pt_i + 1) % 4
                        self.pt_i += 2
                        self.act(self.pT[i0], pown, AF.Exp, [pownr], [("pT", i0)], scale=0.125)
                        self.act(self.pT[i1], pprev, AF.Exp, [pprevr], [("pT", i1)], scale=0.125)
                        stA[gi] = (i0, i1)
                    if gi >= 1:
                        grp = gi - 1
                        i0, i1 = stA.pop(grp)
                        po, por = self.psum_acc()
                        first = True
                        for qb in range(4):
                            ti = grp * 4 + qb
                            nb = ti % nbk
                            cs = slice(qb * 128, (qb + 1) * 128)
                            if nb > 0:
                                self.mm(po[:, cs], Va[:, ti - 1, :], self.pT[i1][:, cs], first, False,
                                        [("Va", b, (ti - 1) // 8), ("pT", i1)], [por], skip=True)
                                first = False
                            self.mm(po[:, cs], Va[:, ti, :], self.pT[i0][:, cs], first, True, [("Va", b, ti // 8), ("pT", i0)], [por], skip=True)
                            first = False
                        for qb in range(4):
                            ti = grp * 4 + qb
                            cs = slice(qb * 128, (qb + 1) * 128)
                            if g == 0:
                                self.cp(acc[:, tok(ti)], po[:, cs], [por], ["acc"])
                            else:
                                self.tt(acc[:, tok(ti)], po[:, cs], acc[:, tok(ti)], ALU.add, [por, "acc"], ["acc"])
            for qt in range(8):
                qs = slice(qt * 512, (qt + 1) * 512)
                self.attn_finish(acc[:, qs], "acc", 768 + j * 64, qs, qt)
        A.release()
        P.barrier()

    def merge(self, l, win, nxt):
        P, A = self.P, self.A
        xs_v = self.xs.rearrange("(c p) t -> p c t", p=128)
        hs_v = self.hs.rearrange("(c p) t -> p c t", p=128)
        br_v = self.brs.rearrange("(c p) t -> p c t", p=128)
        A.mark()
        wbr = A.alloc((8, D), BF16)
        wout = A.alloc((8, D), BF16)
        TG = 1024
        xg = A.alloc((8, TG), F32)
        hg = A.alloc((8, TG), BF16)
        bg = A.alloc((8, TG), BF16)
        mg = A.alloc((8, TG), BF16)
        gs = [A.alloc(512, F32) for _ in range(3)]
        tm = [A.alloc(512, F32) for _ in range(3)]
        self.alloc_norm_bufs(final=(nxt is None))
        wbres, wores = [], []
        wo_d = self.w["w_out"][l].rearrange("(c p) d -> p c d", p=128)
        for hf in range(2):
            hs_ = slice(hf * 512, (hf + 1) * 512)
            for i, (nm, c0, c1) in enumerate((("w_br_mla", 0, 3), ("w_br_moba", 3, 6), ("w_br_dil", 6, 8))):
                self.ld(wbr[:, c0:c1, hs_], self.w[nm][l].rearrange("(c p) d -> p c d", p=128)[:, :, hs_], (), [("wbr", i, hf)], eng="pool")
                wbres.append(("wbr", i, hf))
            for i in range(4):
                self.ld(wout[:, 2 * i:2 * i + 2, hs_], wo_d[:, 2 * i:2 * i + 2, hs_], (), [("wout", i, hf)], eng="pool")
                wores.append(("wout", i, hf))
        gbase = 3872
        brch = ((0, 3), (3, 6), (6, 8))
        for tg in range(S // TG):
            sl = slice(tg * TG, (tg + 1) * TG)
            self.ld(xg, xs_v[:, :, sl], [("xs", tg)], [("gx", 0), ("gx", 1)])
            self.ld(hg, hs_v[:, :, sl], [("hs", 2 * tg), ("hs", 2 * tg + 1)], ["hg"])
            self.ld(bg, br_v[:, :, sl], [], ["bg"])
            for dc in range(8):
                if dc == 1:
                    self.flush_deferred()
                ds_ = slice(dc * 128, (dc + 1) * 128)
                w16, wres = self.wload([(lambda s, i=i: s.rearrange("p (c f) -> p c f", c=8)[:, :, i * 128:(i + 1) * 128],
                                         win[:, :, gbase + i * D + dc * 128:gbase + i * D + dc * 128 + 128]) for i in range(3)])
                w16 = w16.rearrange("p (c f) -> p c f", c=8)
                for st in range(2):
                    ts_ = slice(st * 512, (st + 1) * 512)
                    for i in range(3):
                        pg, pgr = self.psum()
                        for c in range(8):
                            self.mm(pg, w16[:, c, i * 128:(i + 1) * 128], hg[:, c, ts_], c == 0, c == 7, wres + ["hg"], [pgr])
                        self.act(gs[i], pg, AF.Sigmoid, [pgr], [("gs", i)])
                        pb, pbr = self.psum()
                        c0, c1 = brch[i]
                        for c in range(c0, c1):
                            self.mm(pb, wbr[:, c, ds_], bg[:, c, ts_], c == c0, c == c1 - 1, wbres + ["bg"], [pbr])
                        self.tt(tm[i], gs[i], pb, ALU.mult, [("gs", i), pbr], [("tm", i)])
                    self.tt(tm[0], tm[0], tm[1], ALU.add, [("tm", 0), ("tm", 1)], [("tm", 0)])
                    self.tt(mg[:, dc, ts_], tm[0], tm[2], ALU.add, [("tm", 0), ("tm", 2)], [("mg", dc, st)])
            for ec in range(8):
                es_ = slice(ec * 128, (ec + 1) * 128)
                for st in range(2):
                    ts_ = slice(st * 512, (st + 1) * 512)
                    po, por = self.psum()
                    for dc in range(8):
                        self.mm(po, wout[:, dc, es_], mg[:, dc, ts_], dc == 0, dc == 7, wores + [("mg", dc, st)], [por])
                    self.tt(xg[:, ec, ts_], po, xg[:, ec, ts_], ALU.add, [por, ("gx", st)], [("gx", st)])
            if nxt is not None:
                self.ld(xs_v[:, :, sl], xg, [("gx", 0), ("gx", 1)], [("xs", tg)])
            ngb, nl = nxt if nxt is not None else (0, 0)
            for st in range(2):
                self.deferred.append(lambda st=st, tg=tg, ngb=ngb, nl=nl: self.norm_out(
                    xg[:, :, st * 512:(st + 1) * 512], ("gx", st), 2 * tg + st, ngb, nl))
            self.flush_deferred()
        A.release()


def host_consts():
    import ml_dtypes
    bf = lambda a: np.asarray(a, np.float32).astype(ml_dtypes.bfloat16).astype(np.float32)
    t = np.arange(S)
    inv = (np.float32(10000.0) ** (-np.arange(0, 32, 2, dtype=np.float32) / np.float32(32))).astype(np.float32)
    ang = t[:, None].astype(np.float32) * inv[None, :]
    cos = np.cos(ang).T.astype(np.float32)
    sin = np.sin(ang).T.astype(np.float32)
    rope = np.concatenate([cos, cos, -sin, sin], axis=0)
    slopes = (2.0 ** (-8.0 * np.arange(1, 19, dtype=np.float32) / 18)).astype(np.float32)
    s_hi = bf(slopes)
    s_lo = bf(slopes - s_hi)
    a = (t // 64).astype(np.float32)
    b = (t % 64).astype(np.float32)
    one = np.ones(S, np.float32)
    qc = np.zeros((18, 8, S), np.float32)
    kc = np.zeros((18, 8, S), np.float32)
    for h in range(18):
        qc[h] = np.stack([-512 * a, -8 * b, -512 * a, -8 * b, s_hi[h] * one, s_hi[h] * one, s_lo[h] * one, s_lo[h] * one])
        kc[h] = np.stack([s_hi[h] * one, s_hi[h] * one, s_lo[h] * one, s_lo[h] * one, 512 * a, 8 * b, 512 * a, 8 * b])
    onehot = (t[None, :] // 256 == np.arange(16)[:, None]).astype(np.float32)
    NEG = np.float32(-30000.0)
    k = np.arange(128)[:, None]
    c = np.arange(896)[None, :]
    cmask = np.where(c - 384 >= k, np.float32(0), NEG).astype(np.float32)
    q = np.arange(128)[None, :]
    own = np.where(k <= q, np.float32(0), NEG).astype(np.float32)
    prev = np.where(k >= q, np.float32(0), NEG).astype(np.float32)
    dmask = np.stack([np.tile(own, (1, 4)), np.tile(prev, (1, 4))], axis=1)
    qt_i = np.arange(512) // 16
    n_i = np.arange(512) % 16
    bo = qt_i // 2
    selc = np.stack([(n_i < bo).astype(np.float32), np.where(n_i < bo, np.float32(0), np.float32(-1e30)),
                     (n_i != bo).astype(np.float32)], axis=0)
    selc = np.ascontiguousarray(np.broadcast_to(selc[None], (128, 3, 512))).astype(np.float32)
    return {"c_selc": selc, "c_rope": np.ascontiguousarray(rope), "c_qc": qc, "c_kc": kc, "c_onehot": onehot, "c_cmask": cmask,
            "c_dmask": np.ascontiguousarray(dmask), "c_ident": np.eye(128, dtype=np.float32)}


def host_layout(inputs):
    f = lambda a: np.ascontiguousarray(np.asarray(a, dtype=np.float32))
    col = lambda v: f(v).reshape(-1, 128).T
    gains = np.zeros((128, 124), np.float32)
    for l in range(NL):
        gains[:, l * 8:(l + 1) * 8] = col(inputs["ffn1_norm"][l])
        gains[:, 32 + l * 8:32 + (l + 1) * 8] = col(inputs["mix_norm"][l])
        gains[:, 72 + l * 8:72 + (l + 1) * 8] = col(inputs["ffn2_norm"][l])
        gains[:, 64 + l * 2:64 + (l + 1) * 2] = col(inputs["q_norm"][l])
        gains[:, 112 + l:113 + l] = col(inputs["kv_norm"][l])
    gains[:, 104:112] = col(inputs["final_norm"])
    shared = {"gains": gains}
    shared.update(host_consts())
    for nm in ("ffn1_w_gate", "ffn1_w_up", "ffn1_w_down", "ffn2_w_gate", "ffn2_w_up", "ffn2_w_down", "w_in", "w_uq",
               "w_ukv", "w_br_mla", "w_br_moba", "w_br_dil", "w_out"):
        shared[nm] = f(inputs[nm])
    x = np.asarray(inputs["x"], dtype=np.float32)
    maps = []
    for b in range(NCORES):
        m = dict(shared)
        m["xT"] = np.ascontiguousarray(x[b].T)
        maps.append(m)
    return maps


_NC_CACHE = {}


def kernel(**inputs):
    if "nc" not in _NC_CACHE:
        _NC_CACHE["nc"] = Builder().build()
    nc = _NC_CACHE["nc"]
    maps = host_layout(inputs)
    res = run_bass_kernel_spmd(nc, maps, core_ids=list(range(NCORES)))
    out = np.stack([np.ascontiguousarray(res.results[b]["outT"].T) for b in range(NCORES)], axis=0)
    return out.astype(np.float32)
```

```python
import numpy as np
from contextlib import ExitStack
import concourse.bass as bass
import concourse.mybir as mybir
from concourse.bass_utils import run_bass_kernel_spmd

F32 = mybir.dt.float32
BF16 = mybir.dt.bfloat16
AF = mybir.ActivationFunctionType
ALU = mybir.AluOpType
AX = mybir.AxisListType

D = 1024
S = 4096
DFF = 2816
NL = 4
NCORES = 8
EPS = 1e-6
IN_COLS = 6944
ENGS = ("pe", "act", "dve", "pool", "sp")


class Prog:
    def __init__(self, nc, n_dma_sems=48):
        self.nc = nc
        self.q = {e: [] for e in ENGS}
        self.cnt = {e: 0 for e in ENGS}
        self.seen = {e: {} for e in ENGS}
        self.lastw = {}
        self.reads = {}
        self.n_dma_sems = n_dma_sems
        self.dma_i = 0
        self.dma_i_sw = 0
        self.dma_val = [0] * n_dma_sems
        self.sems = {}
        self.local_dma = []

    def _deps(self, eng, reads, writes):
        need = {}

        def add(tok):
            if tok is None:
                return
            k, v = tok
            if k == "pe" and eng == "pe":
                return
            if need.get(k, 0) < v:
                need[k] = v

        for r in reads:
            add(self.lastw.get(r))
        for w in writes:
            add(self.lastw.get(w))
            for k, v in self.reads.get(w, {}).items():
                add((k, v))
        out = []
        for k, v in need.items():
            if self.seen[eng].get(k, 0) < v:
                self.seen[eng][k] = v
                out.append((k, v))
        return out

    def _commit(self, tok, reads, writes):
        k, v = tok
        for r in reads:
            d = self.reads.setdefault(r, {})
            if d.get(k, 0) < v:
                d[k] = v
        for w in writes:
            self.lastw[w] = tok
            self.reads[w] = {}

    def op(self, eng, fn, reads=(), writes=()):
        waits = self._deps(eng, reads, writes)
        self.cnt[eng] += 1
        tok = (eng, self.cnt[eng])
        self.q[eng].append((waits, fn, (eng, 1)))
        self._commit(tok, reads, writes)
        return tok

    def dma(self, fn, reads=(), writes=(), eng="sp", local=True):
        half = self.n_dma_sems // 2
        if eng == "pool":
            i = half + self.dma_i_sw % half
            self.dma_i_sw += 1
        else:
            i = self.dma_i % half
            self.dma_i += 1
        key = ("dma", i)
        waits = self._deps(eng, reads, writes)
        prev = self.dma_val[i]
        if prev and self.seen[eng].get(key, 0) < prev:
            self.seen[eng][key] = prev
            waits.append((key, prev))
        self.dma_val[i] = prev + 16
        tok = (key, prev + 16)
        self.q[eng].append((waits, fn, (key, 16)))
        self._commit(tok, reads, writes)
        if local:
            self.local_dma.append(tok)
        return tok

    def wait_all(self, eng, toks):
        waits = []
        for k, v in toks:
            if self.seen[eng].get(k, 0) < v:
                self.seen[eng][k] = v
                waits.append((k, v))
        if waits:
            self.q[eng].append((waits, None, None))

    def barrier(self):
        toks = [(e, self.cnt[e]) for e in ("pe", "act", "dve") if self.cnt[e]]
        best = {}
        for k, v in self.local_dma:
            if best.get(k, 0) < v:
                best[k] = v
        toks += list(best.items())
        self.local_dma = []
        for e in ENGS:
            self.wait_all(e, [t for t in toks if t[0] != e])

    def emit(self, ctx):
        nc = self.nc
        keys = list(ENGS) + [("dma", i) for i in range(self.n_dma_sems)]
        for k in keys:
            name = k if isinstance(k, str) else f"dma{k[1]}"
            self.sems[k] = ctx.enter_context(nc.semaphore("s_" + name))
        block = ctx.enter_context(nc.Block())
        sems = self.sems

        def run(e, lst):
            for waits, fn, inc in lst:
                for k, v in waits:
                    e.wait_ge(sems[k], v)
                if fn is not None:
                    fn(e).then_inc(sems[inc[0]], inc[1])

        q = self.q

        @block.tensor
        def _(e):
            run(e, q["pe"])

        @block.scalar
        def _(e):
            run(e, q["act"])

        @block.vector
        def _(e):
            run(e, q["dve"])

        @block.gpsimd
        def _(e):
            run(e, q["pool"])

        @block.sync
        def _(e):
            run(e, q["sp"])


class Arena:
    def __init__(self, nc, ctx, nwords=52800):
        self.t = ctx.enter_context(nc.sbuf_tensor("arena", [128, nwords], F32))
        self.nwords = nwords
        self.top = 0
        self.marks = []

    def alloc(self, shape, dt):
        if isinstance(shape, int):
            shape = (shape,)
        n = int(np.prod(shape))
        esz = 4 if dt == F32 else 2
        words = ((n * esz + 3) // 4 + 7) // 8 * 8
        off = self.top
        self.top += words
        assert self.top <= self.nwords, f"SBUF arena overflow {self.top} > {self.nwords}"
        v = self.t[:, off:off + words]
        if dt != F32:
            v = v.bitcast(dt)
        v = v[:, 0:n]
        if len(shape) == 2:
            v = v.rearrange("p (a b) -> p a b", a=shape[0])
        elif len(shape) == 3:
            v = v.rearrange("p (a b c) -> p a b c", a=shape[0], b=shape[1])
        return v

    def mark(self):
        self.marks.append(self.top)

    def release(self):
        self.top = self.marks.pop()


WSLOT = 3072
NSLOT = 6


class Builder:
    def __init__(self, nlayers=NL, phases=("ffn1", "mix", "ffn2"), debug=False, sub=("mla", "moba", "dil", "merge")):
        self.sub = sub
        self.nlayers = nlayers
        self.phases = phases
        self.debug = debug
        self.nc = nc = bass.Bass("TRN2", target_bir_lowering=False)
        self.ctx = ExitStack()
        dk = "ExternalOutput" if debug else "Internal"
        inp = lambda name, shape: nc.dram_tensor(name, list(shape), F32, kind="ExternalInput").ap()
        self.xT = inp("xT", (D, S))
        self.w = {}
        for nm, shp in [("ffn1_w_gate", (NL, D, DFF)), ("ffn1_w_up", (NL, D, DFF)), ("ffn1_w_down", (NL, DFF, D)),
                        ("ffn2_w_gate", (NL, D, DFF)), ("ffn2_w_up", (NL, D, DFF)), ("ffn2_w_down", (NL, DFF, D)),
                        ("w_in", (NL, D, IN_COLS)), ("w_uq", (NL, 256, 576)), ("w_ukv", (NL, 128, 768)),
                        ("w_br_mla", (NL, 384, D)), ("w_br_moba", (NL, 384, D)), ("w_br_dil", (NL, 256, D)),
                        ("w_out", (NL, D, D))]:
            self.w[nm] = inp(nm, shp)
        self.gains_d = inp("gains", (128, 104 + 8 + 12))
        self.c_rope = inp("c_rope", (64, S))
        self.c_qc = inp("c_qc", (18, 8, S))
        self.c_kc = inp("c_kc", (18, 8, S))
        self.c_onehot = inp("c_onehot", (16, S))
        self.c_cmask = inp("c_cmask", (128, 896))
        self.c_dmask = inp("c_dmask", (128, 2, 512))
        self.c_ident = inp("c_ident", (128, 128))
        self.c_selc = inp("c_selc", (128, 3, 512))
        self.hs = nc.dram_tensor("hs", [D, S], BF16, kind=dk).ap()
        self.brs = nc.dram_tensor("brs", [D, S], BF16, kind=dk).ap()
        self.outT = nc.dram_tensor("outT", [D, S], F32, kind="ExternalOutput").ap()
        self.xs = nc.dram_tensor("xs", [D, S], F32, kind=dk).ap()

    def mm(self, out, lhsT, rhs, start, stop, reads, writes, skip=False):
        if skip:
            self.P.op("pe", lambda e: e.matmul(out, lhsT=lhsT, rhs=rhs, start=start, stop=stop, skip_group_check=True), reads, writes)
        else:
            self.P.op("pe", lambda e: e.matmul(out, lhsT=lhsT, rhs=rhs, start=start, stop=stop), reads, writes)

    def act(self, out, in_, func, reads, writes, bias=None, scale=1.0):
        if bias is None:
            self.P.op("act", lambda e: e.activation(out=out, in_=in_, func=func, scale=scale), reads, writes)
        else:
            self.P.op("act", lambda e: e.activation(out=out, in_=in_, func=func, bias=bias, scale=scale), reads, writes)

    def stt(self, out, in0, scalar, in1, op0, op1, reads, writes, eng="dve"):
        self.P.op(eng, lambda e: e.scalar_tensor_tensor(out=out, in0=in0, scalar=scalar, in1=in1, op0=op0, op1=op1), reads, writes)

    def ts(self, out, in0, s1, s2, op0, op1, reads, writes, eng="dve"):
        if s2 is None:
            self.P.op(eng, lambda e: e.tensor_scalar(out=out, in0=in0, scalar1=s1, scalar2=None, op0=op0), reads, writes)
        else:
            self.P.op(eng, lambda e: e.tensor_scalar(out=out, in0=in0, scalar1=s1, scalar2=s2, op0=op0, op1=op1), reads, writes)

    def rsqrt(self, out, in_, reads, writes):
        self.act(out, in_, AF.Sqrt, list(reads) + ["epsc"], list(writes), bias=self.epsc[:, 0:1])
        self.P.op("dve", lambda e: e.reciprocal(out=out, in_=out), list(writes), list(writes))

    def tt(self, out, in0, in1, op, reads, writes, eng="dve"):
        self.P.op(eng, lambda e: e.tensor_tensor(out=out, in0=in0, in1=in1, op=op), reads, writes)

    def cp(self, out, in_, reads, writes, eng="dve"):
        self.P.op(eng, lambda e: e.tensor_copy(out=out, in_=in_), reads, writes)

    def memset(self, ap, val, writes, eng="dve"):
        self.P.op(eng, lambda e: e.memset(ap, val), (), writes)

    def ld(self, out, in_, reads, writes, eng="sp", local=True):
        return self.P.dma(lambda e: e.dma_start(out=out, in_=in_), reads, writes, eng=eng, local=local)

    def wload(self, parts):
        s = self.w_i % NSLOT
        self.w_i += 1
        slot = self.wring[s]
        res = ("wring", s)
        for k, (dstf, src) in enumerate(parts):
            dst = dstf(slot)
            self.P.dma(lambda e, dst=dst, src=src: e.dma_start(out=dst, in_=src), (), [res] if k == 0 else [(res, k)],
                       eng="pool", local=False)
        allres = [res] + [(res, k) for k in range(1, len(parts))]
        return slot, allres

    def psum(self):
        i = 2 + self.ps_i % 6
        self.ps_i += 1
        return self.psb[i][:], ("ps", i)

    def psum_acc(self):
        i = self.pa_i % 2
        self.pa_i += 1
        return self.psb[i][:], ("ps", i)

    def build(self):
        nc, ctx = self.nc, self.ctx
        self.P = P = Prog(nc)
        self.A = A = Arena(nc, ctx)
        self.psb = [ctx.enter_context(nc.psum_tensor(f"psb{i}", [128, 512], F32)) for i in range(8)]
        self.ps_i = 0
        self.pa_i = 0
        self.pt_i = 0
        self.fin_i = 0
        self.w_i = 0
        self.wring = [A.alloc(WSLOT, BF16) for _ in range(NSLOT)]
        self.gains = A.alloc(124, F32)
        self.ones = {n: A.alloc(128, BF16) for n in (1024, 256, 128)}
        self.ld(self.gains, self.gains_d, (), ["gains"])
        self.epsc = A.alloc(8, F32)
        self.memset(self.epsc, EPS, ["epsc"])
        for n, t in self.ones.items():
            self.memset(t, 1.0 / n, [("ones", n)])
        self.ident = A.alloc(128, BF16)
        self.cmask = A.alloc(896, BF16)
        self.dmask = A.alloc((2, 512), BF16)
        self.wkr = A.alloc((8, 2, 96), BF16)
        self.wqB = A.alloc((2, 6, 96), BF16)
        self.ld(self.ident, self.c_ident, (), ["consts"], eng="pool")
        self.ld(self.cmask, self.c_cmask, (), [("consts", 1)], eng="pool")
        self.ld(self.dmask, self.c_dmask, (), [("consts", 2)], eng="pool")
        self.cmask01 = A.alloc(896, BF16)
        self.dmask01 = A.alloc((2, 512), BF16)
        self.ts(self.cmask01, self.cmask, -1.0, None, ALU.is_ge, None, [("consts", 1)], ["m01"])
        self.ts(self.dmask01, self.dmask, -1.0, None, ALU.is_ge, None, [("consts", 2)], ["m01"])
        self.memset(self.wkr, 0.0, ["wkr"])
        self.memset(self.wqB, 0.0, ["wqB"])
        self.deferred = []
        self.final_toks = []
        self.no_i = 0
        P.barrier()
        if self.phases[0] == "ffn1":
            self.pre_norm(0, 0)
        else:
            for i in range(8):
                self.ld(self.xs[i * 128:(i + 1) * 128, :], self.xT[i * 128:(i + 1) * 128, :], (), [("xs", tg) for tg in range(4)])
            self.pre_norm(32, 0)
        P.barrier()
        seq = [(l, ph) for l in range(self.nlayers) for ph in ("ffn1", "mix", "ffn2") if ph in self.phases]
        gb = {"ffn1": 0, "mix": 32, "ffn2": 72}
        for i, (l, ph) in enumerate(seq):
            nxt = (gb[seq[i + 1][1]], seq[i + 1][0]) if i + 1 < len(seq) else None
            if ph == "mix":
                self.mixer(l, nxt)
            else:
                self.ffn(l, ph, gb[ph], nxt)
            P.barrier()
        P.wait_all("sp", self.final_toks)
        with nc.allow_low_precision(reason="bf16 matmul operands, fp32 accumulation (reference tolerance is bf16-level)"):
            P.emit(ctx)
        ctx.close()
        return nc

    def pre_norm(self, gbase, l):
        A = self.A
        xT_v = self.xT.rearrange("(c p) t -> p c t", p=128)
        A.mark()
        xg = [A.alloc((8, 512), F32) for _ in range(2)]
        self.alloc_norm_bufs(False)
        for t in range(8):
            b = t % 2
            self.ld(xg[b], xT_v[:, :, t * 512:(t + 1) * 512], (), [("px", b)])
            self.norm_out(xg[b], ("px", b), t, gbase, l)
        A.release()

    def gcol(self, base, l, c):
        k = base + l * 8 + c
        return self.gains[:, k:k + 1]

    def norm_tile(self, X, xres, hdst, hres, gbase, l, sq, rstd, n=512):
        self.act(sq[:, :, 0:n], X, AF.Square, [xres], ["sq"])
        ps, pr = self.psum()
        for c in range(8):
            self.mm(ps[:, 0:n], self.ones[1024], sq[:, c, 0:n], c == 0, c == 7, ["sq", ("ones", 1024)], [pr])
        self.rsqrt(rstd[:, 0:n], ps[:, 0:n], [pr], ["rstd"])
        for c in range(8):
            self.stt(hdst[:, c, :], X[:, c, :], self.gcol(gbase, l, c), rstd[:, 0:n], ALU.mult, ALU.mult,
                     [xres, "rstd", "gains"], [hres])

    def alloc_norm_bufs(self, final=False):
        A = self.A
        self.no_sq = A.alloc((8, 512), BF16)
        self.no_rstd = A.alloc(512, F32)
        self.no_final = final
        if final:
            self.no_og = A.alloc((8, 512), F32)
        else:
            self.no_hb = [A.alloc((8, 512), BF16) for _ in range(2)]

    def norm_out(self, X, xres, t_idx, gbase, l):
        gsl = slice(t_idx * 512, (t_idx + 1) * 512)
        final = self.no_final
        i = self.no_i % 2
        self.no_i += 1
        dst, dres = (self.no_og, "no_og") if final else (self.no_hb[i], ("no_hb", i))
        self.act(self.no_sq, X, AF.Square, [xres], ["no_sq"])
        ps, pr = self.psum()
        for c in range(8):
            self.mm(ps, self.ones[1024], self.no_sq[:, c, :], c == 0, c == 7, ["no_sq", ("ones", 1024)], [pr])
        self.rsqrt(self.no_rstd, ps, [pr], ["no_rstd"])
        for c in range(8):
            k = 104 + c if final else gbase + l * 8 + c
            self.stt(dst[:, c, :], X[:, c, :], self.gains[:, k:k + 1], self.no_rstd, ALU.mult, ALU.mult,
                     [xres, "no_rstd", "gains"], [dres])
        if final:
            out_v = self.outT.rearrange("(c p) t -> p c t", p=128)
            self.final_toks.append(self.ld(out_v[:, :, gsl], dst, [dres], [("out", t_idx)]))
        else:
            hs_v = self.hs.rearrange("(c p) t -> p c t", p=128)
            self.ld(hs_v[:, :, gsl], dst, [dres], [("hs", t_idx)])

    def flush_deferred(self):
        for f in self.deferred:
            f()
        self.deferred = []

    def ffn(self, l, name, gbase, nxt):
        P, A = self.P, self.A
        wg = self.w[name + "_w_gate"][l].rearrange("(c p) f -> p c f", p=128)
        wu = self.w[name + "_w_up"][l].rearrange("(c p) f -> p c f", p=128)
        wd = self.w[name + "_w_down"][l].rearrange("(c p) d -> p c d", p=128)
        xs_v = self.xs.rearrange("(c p) t -> p c t", p=128)
        hs_v = self.hs.rearrange("(c p) t -> p c t", p=128)
        src_v = self.xT.rearrange("(c p) t -> p c t", p=128) if (l == 0 and name == "ffn1") else xs_v
        A.mark()
        TG = 1024
        xg = [A.alloc((8, TG), F32) for _ in range(2)]
        hT = A.alloc((8, TG), BF16)
        actT = A.alloc((22, TG), BF16)
        sg = [A.alloc(512, BF16) for _ in range(2)]
        self.alloc_norm_bufs(final=(nxt is None))
        def load_group(tg):
            sl_ = slice(tg * TG, (tg + 1) * TG)
            self.ld(xg[tg % 2], src_v[:, :, sl_], [("xs", tg)], [("xg", tg % 2, 0), ("xg", tg % 2, 1)])
            self.ld(hT, hs_v[:, :, sl_], [("hs", 2 * tg), ("hs", 2 * tg + 1)], [("hT", 0), ("hT", 1)])

        load_group(0)
        for tg in range(S // TG):
            b = tg % 2
            X = xg[b]
            sl = slice(tg * TG, (tg + 1) * TG)
            k = 0
            for fp in range(11):
                if fp == 2:
                    self.flush_deferred()
                fs = slice(fp * 256, (fp + 1) * 256)
                g16, gres = self.wload([(lambda s: s.rearrange("p (c f) -> p c f", c=8)[:, :, 0:256], wg[:, :, fs])])
                u16, ures = self.wload([(lambda s: s.rearrange("p (c f) -> p c f", c=8)[:, :, 0:256], wu[:, :, fs])])
                g16 = g16.rearrange("p (c f) -> p c f", c=8)
                u16 = u16.rearrange("p (c f) -> p c f", c=8)
                for half in range(2):
                    fc = fp * 2 + half
                    hs = slice(half * 128, (half + 1) * 128)
                    for st in range(2):
                        ts_ = slice(st * 512, (st + 1) * 512)
                        pg, pgr = self.psum()
                        for c in range(8):
                            self.mm(pg, g16[:, c, hs], hT[:, c, ts_], c == 0, c == 7, gres + [("hT", st)], [pgr])
                        pu, pur = self.psum()
                        for c in range(8):
                            self.mm(pu, u16[:, c, hs], hT[:, c, ts_], c == 0, c == 7, ures + [("hT", st)], [pur])
                        s_ = sg[k % 2]
                        self.act(s_, pg, AF.Silu, [pgr], [("sg", k % 2)])
                        self.tt(actT[:, fc, ts_], s_, pu, ALU.mult, [("sg", k % 2), pur], [("actT", fc, st)])
                        k += 1
            if tg + 1 < S // TG:
                load_group(tg + 1)
            for dc in range(8):
                ds_ = slice(dc * 128, (dc + 1) * 128)
                d16, dres = self.wload([(lambda s: s.rearrange("p (c f) -> p c f", c=24)[:, 0:22, :], wd[:, :, ds_])])
                d16 = d16.rearrange("p (c f) -> p c f", c=24)
                for st in range(2):
                    ts_ = slice(st * 512, (st + 1) * 512)
                    po, por = self.psum()
                    for fc in range(22):
                        self.mm(po, d16[:, fc, :], actT[:, fc, ts_], fc == 0, fc == 21, dres + [("actT", fc, st)], [por])
                    self.stt(X[:, dc, ts_], po, 0.5, X[:, dc, ts_], ALU.mult, ALU.add, [por, ("xg", b, st)], [("xg", b, st)])
            if nxt is not None:
                self.ld(xs_v[:, :, sl], X, [("xg", b, 0), ("xg", b, 1)], [("xs", tg)])
            ngb, nl = nxt if nxt is not None else (0, 0)
            for st in range(2):
                self.deferred.append(lambda X=X, b=b, st=st, tg=tg, ngb=ngb, nl=nl: self.norm_out(
                    X[:, :, st * 512:(st + 1) * 512], ("xg", b, st), 2 * tg + st, ngb, nl))
        self.flush_deferred()
        A.release()

    def load_hT(self, hT):
        hs_v = self.hs.rearrange("(c p) t -> p c t", p=128)
        for t in range(8):
            sl = slice(t * 512, (t + 1) * 512)
            self.ld(hT[:, :, sl], hs_v[:, :, sl], [("hs", t)], [("hT", t)])

    def attn_finish(self, acc_ap, accres, row0, qs, qt):
        i = self.fin_i % 2
        self.fin_i += 1
        osb, rd, ob = self.osb[i], self.rd[i], self.ob[i]
        self.cp(osb, acc_ap, [accres], [("osb", i)])
        self.P.op("dve", lambda e: e.reciprocal(out=rd[64:128, :], in_=osb[64:128, :]), [("osb", i)], [("rd", i)])
        pr, prr = self.psum()
        self.mm(pr[0:64, :], self.ident[64:128, 64:128], rd[64:128, :], True, True, [("rd", i), "consts"], [prr])
        self.tt(ob[0:64, :], osb[0:64, :], pr[0:64, :], ALU.mult, [("osb", i), prr], [("ob", i)])
        self.ld(self.brs[row0:row0 + 64, qs], ob[0:64, :], [("ob", i)], [("brs", row0, qt)])

    def attn_causal(self, Qa, Ka, Va, qres, kres, vres, KD, scale, row0):
        LA = 2
        pairs = [(qt, kt) for qt in range(8) for kt in range(4 * qt + 4)]
        st = {}
        pos = {}
        for i in range(len(pairs) + LA):
            if i < len(pairs):
                qt, kt = pairs[i]
                qs = slice(qt * 512, (qt + 1) * 512)
                ps, psr = self.psum()
                diag = kt >= 4 * qt
                self.mm(ps, Ka[0:KD, kt * 128:(kt + 1) * 128], Qa[0:KD, qs], True, True, kres(kt) + qres(qt), [psr])
                pi = self.pt_i % 4
                self.pt_i += 1
                self.act(self.pT[pi], ps, AF.Exp, [psr], [("pT", pi)], scale=scale)
                if diag:
                    jj = kt - 4 * qt
                    c0 = 384 - 128 * jj
                    self.tt(self.pT[pi], self.pT[pi], self.cmask01[:, c0:c0 + 512], ALU.mult, [("pT", pi), "m01"], [("pT", pi)])
                st[i] = pi
            j = i - LA
            if j >= 0:
                qt, kt = pairs[j]
                nk = 4 * qt + 4
                if kt == 0:
                    pos[qt] = self.psum_acc()
                po, por = pos[qt]
                pi = st.pop(j)
                self.mm(po, Va[:, kt, :], self.pT[pi], kt == 0, kt == nk - 1, vres(kt) + [("pT", pi)], [por])
                if kt == nk - 1:
                    self.attn_finish(po, por, row0, slice(qt * 512, (qt + 1) * 512), qt)
            yield

    def alloc_attn_bufs(self):
        A = self.A
        self.Qa = [A.alloc(S, BF16) for _ in range(2)]
        self.Ka = [A.alloc(S, BF16) for _ in range(2)]
        self.Va = [A.alloc((32, 128), BF16) for _ in range(2)]
        self.pT = [A.alloc(512, BF16) for _ in range(4)]
        self.osb = [A.alloc(512, F32) for _ in range(2)]
        self.rd = [A.alloc(512, BF16) for _ in range(2)]
        self.ob = [A.alloc(512, BF16) for _ in range(2)]
        for b in range(2):
            self.memset(self.Va[b][:, :, 64:128], 1.0, [("Va1", b)])
            self.memset(self.Qa[b], 0.0, [("Qa0", b)])
            self.memset(self.Ka[b], 0.0, [("Ka0", b)])

    def proj_qkv(self, w16, wres, hT, Qa_b, Ka_b, Va_b, vT, b, tokfn):
        for t in range(8):
            ts_ = slice(t * 512, (t + 1) * 512)
            ps, psr = self.psum()
            for c in range(8):
                self.mm(ps[0:64, :], w16[:, c, 0:64], hT[:, c, ts_], c == 0, c == 7, wres + [("hT", t)], [psr])
            self.act(Qa_b[0:64, ts_], ps[0:64, :], AF.Copy, [psr, ("Qa0", b)], [("Qa", b, "q", t)])
            yield
            ps, psr = self.psum()
            for c in range(8):
                self.mm(ps, w16[:, c, 64:192], hT[:, c, ts_], c == 0, c == 7, wres + [("hT", t)], [psr])
            self.act(Ka_b[0:64, ts_], ps[0:64, :], AF.Copy, [psr, ("Ka0", b)], [("Ka", b, "q", t)])
            self.cp(vT[64:128, ts_], ps[64:128, :], [psr], [("vT", t)])
            yield
        vres = [("vT", t) for t in range(8)] + ["consts"]
        for t8 in range(4):
            ps, psr = self.psum()
            for j in range(8):
                ti = t8 * 8 + j
                self.mm(ps[:, j * 64:(j + 1) * 64], vT[64:128, tokfn(ti)], self.ident[64:128, 64:128], j == 0, True, vres, [psr], skip=True)
            self.cp(Va_b[:, t8 * 8:(t8 + 1) * 8, 0:64], ps.rearrange("p (a b) -> p a b", a=8), [psr, ("Va1", b)], [("Va", b, t8)])
            yield

    def mixer(self, l, nxt):
        P, A = self.P, self.A
        win = self.w["w_in"][l].rearrange("(c p) k -> p c k", p=128)
        xs_v = self.xs.rearrange("(c p) t -> p c t", p=128)
        hs_v = self.hs.rearrange("(c p) t -> p c t", p=128)
        v192 = lambda s: s.rearrange("p (c f) -> p c f", c=8)[:, :, 0:192]

        if "mla" in self.sub:
            self.mla(l, win)
        if "moba" in self.sub:
            self.moba(l, win, v192)
        if "dil" in self.sub:
            self.dil(l, win, v192)
        if "merge" in self.sub:
            self.merge(l, win, nxt)

    def mla(self, l, win):
        P, A = self.P, self.A
        A.mark()
        cqn = A.alloc((2, S), BF16)
        ckvn = A.alloc(S, BF16)
        krot = A.alloc(S, BF16)
        cosT = A.alloc(S, BF16)
        sinT = A.alloc(S, BF16)
        self.ld(cosT[64:96, :], self.c_rope[0:32, :], (), ["cosT"], eng="pool")
        self.ld(sinT[64:96, :], self.c_rope[32:64, :], (), ["sinT"], eng="pool")
        A.mark()
        hT = A.alloc((8, S), BF16)
        tmp32 = A.alloc((3, 512), F32)
        sq3 = A.alloc((3, 512), BF16)
        rstd2 = A.alloc((2, 512), F32)
        t1 = A.alloc(512, F32)
        t2 = A.alloc(512, F32)
        self.load_hT(hT)
        wlat, wres = self.wload([(lambda s: s.rearrange("p (c f) -> p c f", c=8), win[:, :, 0:384])])
        wlat = wlat.rearrange("p (c f) -> p c f", c=8)
        self.ld(self.wkr[:, :, 0, 64:96], win[:, :, 384:416], (), ["wkr"], eng="pool")
        self.ld(self.wkr[:, :, 1, 64:80], win[:, :, 400:416], (), [("wkr", 1)], eng="pool")
        self.ld(self.wkr[:, :, 1, 80:96], win[:, :, 384:400], (), [("wkr", 2)], eng="pool")
        wkres = ["wkr", ("wkr", 1), ("wkr", 2)]
        for t in range(8):
            ts_ = slice(t * 512, (t + 1) * 512)
            for j in range(3):
                ps, psr = self.psum()
                for c in range(8):
                    self.mm(ps, wlat[:, c, j * 128:(j + 1) * 128], hT[:, c, ts_], c == 0, c == 7, wres + [("hT", t)], [psr])
                self.act(tmp32[:, j, :], ps, AF.Copy, [psr], [("tmp32", j)])
            self.act(sq3, tmp32, AF.Square, [("tmp32", j) for j in range(3)], ["sq3"])
            ps, psr = self.psum()
            for j in range(2):
                self.mm(ps, self.ones[256], sq3[:, j, :], j == 0, j == 1, ["sq3", ("ones", 256)], [psr])
            self.rsqrt(rstd2[:, 0, :], ps, [psr], [("rstd2", 0)])
            ps, psr = self.psum()
            self.mm(ps, self.ones[128], sq3[:, 2, :], True, True, ["sq3", ("ones", 128)], [psr])
            self.rsqrt(rstd2[:, 1, :], ps, [psr], [("rstd2", 1)])
            for j in range(2):
                k = 64 + l * 2 + j
                self.stt(cqn[:, j, ts_], tmp32[:, j, :], self.gains[:, k:k + 1], rstd2[:, 0, :], ALU.mult, ALU.mult,
                         [("tmp32", j), ("rstd2", 0), "gains"], [("cqn", t)])
            k = 112 + l
            self.stt(ckvn[:, ts_], tmp32[:, 2, :], self.gains[:, k:k + 1], rstd2[:, 1, :], ALU.mult, ALU.mult,
                     [("tmp32", 2), ("rstd2", 1), "gains"], [("ckvn", t)])
            pa, par = self.psum()
            for c in range(8):
                self.mm(pa[0:96, :], self.wkr[:, c, 0, :], hT[:, c, ts_], c == 0, c == 7, wkres + [("hT", t)], [par])
            pb, pbr = self.psum()
            for c in range(8):
                self.mm(pb[0:96, :], self.wkr[:, c, 1, :], hT[:, c, ts_], c == 0, c == 7, wkres + [("hT", t)], [pbr])
            self.tt(t1[64:96, :], pa[64:96, :], cosT[64:96, ts_], ALU.mult, [par, "cosT"], ["t1"])
            self.tt(t2[64:96, :], pb[64:96, :], sinT[64:96, ts_], ALU.mult, [pbr, "sinT"], ["t2"])
            self.tt(krot[64:96, ts_], t1[64:96, :], t2[64:96, :], ALU.add, ["t1", "t2"], ["krot"])
        A.release()
        P.barrier()
        A.mark()
        self.alloc_attn_bufs()
        t1 = A.alloc(512, F32)
        t2 = A.alloc(512, F32)
        wuq_d = self.w["w_uq"][l].rearrange("(c p) k -> p c k", p=128)
        wuq, wuqres = self.wload([(lambda s: s[:, 0:1152].rearrange("p (c f) -> p c f", c=2), wuq_d)])
        wuq = wuq[:, 0:1152].rearrange("p (c f) -> p c f", c=2)
        wukv, wukvres = self.wload([(lambda s: s[:, 0:768], self.w["w_ukv"][l])])
        wqres = []
        for h in range(6):
            self.ld(self.wqB[:, :, h, 64:80], wuq_d[:, :, h * 96 + 80:h * 96 + 96], (), [("wqB", h, 0)], eng="pool")
            self.ld(self.wqB[:, :, h, 80:96], wuq_d[:, :, h * 96 + 64:h * 96 + 80], (), [("wqB", h, 1)], eng="pool")
        self.run_heads(6, lambda h: self.mla_prep(h, cqn, ckvn, krot, cosT, sinT, t1, t2, wuq, wuqres, wukv, wukvres),
                       lambda h: self.attn_causal(self.Qa[h % 2], self.Ka[h % 2], self.Va[h % 2],
                                                  lambda qt, b=h % 2: [("Qa", b, "q", qt), ("Qa", b, "r", qt)],
                                                  lambda kt, b=h % 2: [("Ka", b, "q", kt // 4), ("Ka", b, "r")],
                                                  lambda kt, b=h % 2: [("Va", b, kt // 8)],
                                                  96, 96.0 ** -0.5, h * 64), every=3)
        A.release()
        A.release()
        P.barrier()

    def mla_prep(self, h, cqn, ckvn, krot, cosT, sinT, t1, t2, wuq, wuqres, wukv, wukvres):
        if True:
            b = h % 2
            Qa, Ka, Va = self.Qa[b], self.Ka[b], self.Va[b]
            wqr = ["wqB", ("wqB", h, 0), ("wqB", h, 1)]
            for t in range(8):
                ts_ = slice(t * 512, (t + 1) * 512)
                p1, p1r = self.psum()
                for c in range(2):
                    self.mm(p1[0:96, :], wuq[:, c, h * 96:(h + 1) * 96], cqn[:, c, ts_], c == 0, c == 1, wuqres + [("cqn", t)], [p1r])
                p2, p2r = self.psum()
                for c in range(2):
                    self.mm(p2[0:96, :], self.wqB[:, c, h, :], cqn[:, c, ts_], c == 0, c == 1, wqr + [("cqn", t)], [p2r])
                self.act(Qa[0:64, ts_], p1[0:64, :], AF.Copy, [p1r, ("Qa0", b)], [("Qa", b, "q", t)])
                self.tt(t1[64:96, :], p1[64:96, :], cosT[64:96, ts_], ALU.mult, [p1r, "cosT"], ["t1"])
                self.tt(t2[64:96, :], p2[64:96, :], sinT[64:96, ts_], ALU.mult, [p2r, "sinT"], ["t2"])
                self.tt(Qa[64:96, ts_], t1[64:96, :], t2[64:96, :], ALU.add, ["t1", "t2", ("Qa0", b)], [("Qa", b, "r", t)])
                pk, pkr = self.psum()
                self.mm(pk[0:64, :], wukv[:, h * 128:h * 128 + 64], ckvn[:, ts_], True, True, wukvres + [("ckvn", t)], [pkr])
                self.act(Ka[0:64, ts_], pk[0:64, :], AF.Copy, [pkr, ("Ka0", b)], [("Ka", b, "q", t)])
                yield
            self.cp(Ka[64:96, :], krot[64:96, :], ["krot", ("Ka0", b)], [("Ka", b, "r")])
            for t8 in range(4):
                ps, psr = self.psum()
                for j in range(8):
                    kt = t8 * 8 + j
                    self.mm(ps[:, j * 64:(j + 1) * 64], ckvn[:, kt * 128:(kt + 1) * 128], wukv[:, h * 128 + 64:h * 128 + 128],
                            j == 0, True, wukvres + [("ckvn", kt // 4)], [psr], skip=True)
                self.cp(Va[:, t8 * 8:(t8 + 1) * 8, 0:64], ps.rearrange("p (a b) -> p a b", a=8), [psr, ("Va1", b)], [("Va", b, t8)])
                yield

    def moba(self, l, win, v192):
        P, A = self.P, self.A
        A.mark()
        hT = A.alloc((8, S), BF16)
        self.load_hT(hT)
        self.alloc_attn_bufs()
        km32 = A.alloc(16, F32)
        km16 = A.alloc(16, BF16)
        g0 = A.alloc(512, F32)
        g1 = A.alloc(512, F32)
        g2 = A.alloc(512, F32)
        eq = A.alloc(512, F32)
        mx = A.alloc(32, F32)
        selc = A.alloc((3, 512), F32)
        stgb = A.alloc((32, 80), BF16)
        vT = A.alloc(S, BF16)
        self.memset(stgb, 0.0, ["stgb"])
        self.ld(selc, self.c_selc, (), ["selc"])
        base = 416
        self.run_heads(6, lambda h: self.moba_prep(h, hT, win, v192, base, (km32, km16, g0, g1, g2, eq, mx, selc, stgb, vT)),
                       lambda h: self.moba_attend(h))
        A.release()
        P.barrier()

    def run_heads(self, n, prep, attend, every=2):
        for _ in prep(0):
            pass
        for h in range(n):
            nxt = prep(h + 1) if h + 1 < n else None
            for k, _ in enumerate(attend(h)):
                if nxt is not None and k % every == every - 1:
                    next(nxt, None)
            if nxt is not None:
                for _ in nxt:
                    pass

    def moba_attend(self, h):
        b = h % 2
        return self.attn_causal(self.Qa[b], self.Ka[b], self.Va[b],
                                lambda qt, b=b: [("Qa", b, "q", qt), ("Qa", b, "c"), ("Qa", b, "s", qt)],
                                lambda kt, b=b: [("Ka", b, "q", kt // 4), ("Ka", b, "c"), ("Ka", b, "c2")],
                                lambda kt, b=b: [("Va", b, kt // 8)],
                                88, 0.125, 384 + h * 64)

    def moba_prep(self, h, hT, win, v192, base, bufs):
        km32, km16, g0, g1, g2, eq, mx, selc, stgb, vT = bufs
        if True:
            b = h % 2
            Qa, Ka, Va = self.Qa[b], self.Ka[b], self.Va[b]
            w16, wres = self.wload([(lambda s: v192(s)[:, :, 0:64], win[:, :, base + h * 64:base + h * 64 + 64]),
                                    (lambda s: v192(s)[:, :, 64:128], win[:, :, base + 384 + h * 64:base + 384 + h * 64 + 64]),
                                    (lambda s: v192(s)[:, :, 128:192], win[:, :, base + 768 + h * 64:base + 768 + h * 64 + 64])])
            w16 = v192(w16)
            self.ld(Qa[80:88, :], self.c_qc[12 + h], [("Qa0", b)], [("Qa", b, "c")], eng="pool")
            self.ld(Ka[64:80, :], self.c_onehot, [("Ka0", b)], [("Ka", b, "c")], eng="pool")
            self.ld(Ka[80:88, :], self.c_kc[12 + h], [("Ka0", b)], [("Ka", b, "c2")], eng="pool")
            yield
            yield from self.proj_qkv(w16, wres, hT, Qa, Ka, Va, vT, b, lambda ti: slice(ti * 128, (ti + 1) * 128))
            self.P.op("dve", lambda e, Ka=Ka: e.tensor_reduce(out=km32[0:64, :], in_=Ka[0:64, :].rearrange("p (n k) -> p n k", k=256),
                                                             axis=AX.X, op=ALU.add),
                      [("Ka", b, "q", t) for t in range(8)], ["km32"])
            self.ts(km16[0:64, :], km32[0:64, :], 1.0 / 256.0, None, ALU.mult, None, ["km32"], ["km16"])
            yield
            pg, pgr = self.psum()
            for qt in range(32):
                self.mm(pg[:, qt * 16:(qt + 1) * 16], Qa[0:64, qt * 128:(qt + 1) * 128], km16[0:64, :], qt == 0, True,
                        [("Qa", b, "q", qt // 4), "km16"], [pgr], skip=True)
            v3 = lambda ap: ap.rearrange("p (a n) -> p a n", n=16)
            bc = lambda ap: ap.unsqueeze(2).to_broadcast([128, 32, 16])
            self.tt(g0, pg, selc[:, 0, :], ALU.mult, [pgr, "selc"], ["g0"])
            yield
            self.tt(g0, g0, selc[:, 1, :], ALU.add, ["g0", "selc"], ["g0"])
            src = g0
            for rnd, gdst in enumerate((g1, g2)):
                self.P.op("dve", lambda e, src=src: e.tensor_reduce(out=mx, in_=v3(src), axis=AX.X, op=ALU.max), ["g0", "g1", "g2"], ["mx"])
                self.tt(v3(eq), v3(src), bc(mx), ALU.is_ge, ["mx", "g0", "g1", "g2"], ["eq"])
                self.stt(gdst, eq, -2e30, src, ALU.mult, ALU.add, ["eq", "g0", "g1", "g2"], ["g1", "g2"])
                src = gdst
            self.P.op("dve", lambda e: e.tensor_reduce(out=mx, in_=v3(g2), axis=AX.X, op=ALU.max), ["g0", "g1", "g2"], ["mx"])
            self.tt(v3(eq), v3(g0), bc(mx), ALU.is_ge, ["mx", "g0"], ["eq"])
            self.ts(eq, eq, 1.0, 30000.0, ALU.subtract, ALU.mult, ["eq"], ["eq"])
            self.tt(stgb[:, :, 64:80], v3(eq), v3(selc[:, 2, :]), ALU.mult, ["eq", "selc"], ["stgb"])
            for q4 in range(8):
                pt, ptr = self.psum()
                for j in range(4):
                    qt = q4 * 4 + j
                    self.mm(pt[0:80, j * 128:(j + 1) * 128], stgb[:, qt, :], self.ident, j == 0, True, ["stgb", "consts"], [ptr], skip=True)
                self.act(Qa[64:80, q4 * 512:(q4 + 1) * 512], pt[64:80, :], AF.Copy, [ptr, ("Qa0", b)], [("Qa", b, "s", q4)])
                yield

    def dil(self, l, win, v192):
        P, A = self.P, self.A
        A.mark()
        hT = A.alloc((8, S), BF16)
        self.load_hT(hT)
        self.alloc_attn_bufs()
        acc = A.alloc(S, F32)
        vT = A.alloc(S, BF16)
        base = 1568
        idx = 0
        for j in range(4):
            for g in range(3):
                dl = (1, 4, 16)[g]
                nbk = 32 // dl
                hd = 4 * g + j
                b = idx % 2
                idx += 1
                Qa, Ka, Va = self.Qa[b], self.Ka[b], self.Va[b]
                w16, wres = self.wload([(lambda s: v192(s)[:, :, 0:64], win[:, :, base + hd * 64:base + hd * 64 + 64]),
                                        (lambda s: v192(s)[:, :, 64:128], win[:, :, base + 768 + hd * 64:base + 768 + hd * 64 + 64]),
                                        (lambda s: v192(s)[:, :, 128:192], win[:, :, base + 1536 + hd * 64:base + 1536 + hd * 64 + 64])])
                w16 = v192(w16)
                self.ld(Qa[64:72, :], self.c_qc[hd], [("Qa0", b)], [("Qa", b, "c")], eng="pool")
                self.ld(Ka[64:72, :], self.c_kc[hd], [("Ka0", b)], [("Ka", b, "c")], eng="pool")

                def tok(ti, dl=dl, nbk=nbk):
                    r, nb = divmod(ti, nbk)
                    s0 = r + dl * 128 * nb
                    return slice(s0, s0 + dl * 127 + 1, dl)

                for _ in self.proj_qkv(w16, wres, hT, Qa, Ka, Va, vT, b, tok):
                    pass
                qall = [("Qa", b, "q", t) for t in range(8)] + [("Qa", b, "c")]
                kall = [("Ka", b, "q", t) for t in range(8)] + [("Ka", b, "c")]
                stA = {}
                for gi in range(9):
                    if gi < 8:
                        grp = gi
                        pown, pownr = self.psum()
                        pprev, pprevr = self.psum()
                        self.mm(pown, self.ident, self.dmask[:, 0, :], True, False, ["consts", ("consts", 2)], [pownr])
                        self.mm(pprev, self.ident, self.dmask[:, 1, :], True, False, ["consts", ("consts", 2)], [pprevr])
                        for qb in range(4):
                            ti = grp * 4 + qb
                            nb = ti % nbk
                            cs = slice(qb * 128, (qb + 1) * 128)
                            self.mm(pown[:, cs], Ka[0:72, tok(ti)], Qa[0:72, tok(ti)], False, True, qall + kall, [pownr], skip=True)
                            tp = ti - 1 if nb > 0 else ti
                            self.mm(pprev[:, cs], Ka[0:72, tok(tp)], Qa[0:72, tok(ti)], False, True, qall + kall, [pprevr], skip=True)
                        i0 = self.pt_i % 4
                        i1 = (self.pt_i + 1) % 4
                        self.pt_i += 2
                        self.act(self.pT[i0], pown, AF.Exp, [pownr], [("pT", i0)], scale=0.125)
                        self.act(self.pT[i1], pprev, AF.Exp, [pprevr], [("pT", i1)], scale=0.125)
                        stA[gi] = (i0, i1)
                    if gi >= 1:
                        grp = gi - 1
                        i0, i1 = stA.pop(grp)
                        po, por = self.psum_acc()
                        first = True
                        for qb in range(4):
                            ti = grp * 4 + qb
                            nb = ti % nbk
                            cs = slice(qb * 128, (qb + 1) * 128)
                            if nb > 0:
                                self.mm(po[:, cs], Va[:, ti - 1, :], self.pT[i1][:, cs], first, False,
                                        [("Va", b, (ti - 1) // 8), ("pT", i1)], [por], skip=True)
                                first = False
                            self.mm(po[:, cs], Va[:, ti, :], self.pT[i0][:, cs], first, True, [("Va", b, ti // 8), ("pT", i0)], [por], skip=True)
                            first = False
                        for qb in range(4):
                            ti = grp * 4 + qb
                            cs = slice(qb * 128, (qb + 1) * 128)
                            if g == 0:
                                self.cp(acc[:, tok(ti)], po[:, cs], [por], ["acc"])
                            else:
                                self.tt(acc[:, tok(ti)], po[:, cs], acc[:, tok(ti)], ALU.add, [por, "acc"], ["acc"])
            for qt in range(8):
                qs = slice(qt * 512, (qt + 1) * 512)
                self.attn_finish(acc[:, qs], "acc", 768 + j * 64, qs, qt)
        A.release()
        P.barrier()

    def merge(self, l, win, nxt):
        P, A = self.P, self.A
        xs_v = self.xs.rearrange("(c p) t -> p c t", p=128)
        hs_v = self.hs.rearrange("(c p) t -> p c t", p=128)
        br_v = self.brs.rearrange("(c p) t -> p c t", p=128)
        A.mark()
        wbr = A.alloc((8, D), BF16)
        wout = A.alloc((8, D), BF16)
        TG = 1024
        xg = A.alloc((8, TG), F32)
        hg = A.alloc((8, TG), BF16)
        bg = A.alloc((8, TG), BF16)
        mg = A.alloc((8, TG), BF16)
        gs = [A.alloc(512, F32) for _ in range(3)]
        tm = [A.alloc(512, F32) for _ in range(3)]
        self.alloc_norm_bufs(final=(nxt is None))
        wbres, wores = [], []
        wo_d = self.w["w_out"][l].rearrange("(c p) d -> p c d", p=128)
        for hf in range(2):
            hs_ = slice(hf * 512, (hf + 1) * 512)
            for i, (nm, c0, c1) in enumerate((("w_br_mla", 0, 3), ("w_br_moba", 3, 6), ("w_br_dil", 6, 8))):
                self.ld(wbr[:, c0:c1, hs_], self.w[nm][l].rearrange("(c p) d -> p c d", p=128)[:, :, hs_], (), [("wbr", i, hf)], eng="pool")
                wbres.append(("wbr", i, hf))
            for i in range(4):
                self.ld(wout[:, 2 * i:2 * i + 2, hs_], wo_d[:, 2 * i:2 * i + 2, hs_], (), [("wout", i, hf)], eng="pool")
                wores.append(("wout", i, hf))
        gbase = 3872
        brch = ((0, 3), (3, 6), (6, 8))
        for tg in range(S // TG):
            sl = slice(tg * TG, (tg + 1) * TG)
            self.ld(xg, xs_v[:, :, sl], [("xs", tg)], [("gx", 0), ("gx", 1)])
            self.ld(hg, hs_v[:, :, sl], [("hs", 2 * tg), ("hs", 2 * tg + 1)], ["hg"])
            self.ld(bg, br_v[:, :, sl], [], ["bg"])
            for dc in range(8):
                if dc == 1:
                    self.flush_deferred()
                ds_ = slice(dc * 128, (dc + 1) * 128)
                w16, wres = self.wload([(lambda s, i=i: s.rearrange("p (c f) -> p c f", c=8)[:, :, i * 128:(i + 1) * 128],
                                         win[:, :, gbase + i * D + dc * 128:gbase + i * D + dc * 128 + 128]) for i in range(3)])
                w16 = w16.rearrange("p (c f) -> p c f", c=8)
                for st in range(2):
                    ts_ = slice(st * 512, (st + 1) * 512)
                    for i in range(3):
                        pg, pgr = self.psum()
                        for c in range(8):
                            self.mm(pg, w16[:, c, i * 128:(i + 1) * 128], hg[:, c, ts_], c == 0, c == 7, wres + ["hg"], [pgr])
                        self.act(gs[i], pg, AF.Sigmoid, [pgr], [("gs", i)])
                        pb, pbr = self.psum()
                        c0, c1 = brch[i]
                        for c in range(c0, c1):
                            self.mm(pb, wbr[:, c, ds_], bg[:, c, ts_], c == c0, c == c1 - 1, wbres + ["bg"], [pbr])
                        self.tt(tm[i], gs[i], pb, ALU.mult, [("gs", i), pbr], [("tm", i)])
                    self.tt(tm[0], tm[0], tm[1], ALU.add, [("tm", 0), ("tm", 1)], [("tm", 0)])
                    self.tt(mg[:, dc, ts_], tm[0], tm[2], ALU.add, [("tm", 0), ("tm", 2)], [("mg", dc, st)])
            for ec in range(8):
                es_ = slice(ec * 128, (ec + 1) * 128)
                for st in range(2):
                    ts_ = slice(st * 512, (st + 1) * 512)
                    po, por = self.psum()
                    for dc in range(8):
                        self.mm(po, wout[:, dc, es_], mg[:, dc, ts_], dc == 0, dc == 7, wores + [("mg", dc, st)], [por])
                    self.tt(xg[:, ec, ts_], po, xg[:, ec, ts_], ALU.add, [por, ("gx", st)], [("gx", st)])
            if nxt is not None:
                self.ld(xs_v[:, :, sl], xg, [("gx", 0), ("gx", 1)], [("xs", tg)])
            ngb, nl = nxt if nxt is not None else (0, 0)
            for st in range(2):
                self.deferred.append(lambda st=st, tg=tg, ngb=ngb, nl=nl: self.norm_out(
                    xg[:, :, st * 512:(st + 1) * 512], ("gx", st), 2 * tg + st, ngb, nl))
            self.flush_deferred()
        A.release()


def host_consts():
    import ml_dtypes
    bf = lambda a: np.asarray(a, np.float32).astype(ml_dtypes.bfloat16).astype(np.float32)
    t = np.arange(S)
    inv = (np.float32(10000.0) ** (-np.arange(0, 32, 2, dtype=np.float32) / np.float32(32))).astype(np.float32)
    ang = t[:, None].astype(np.float32) * inv[None, :]
    cos = np.cos(ang).T.astype(np.float32)
    sin = np.sin(ang).T.astype(np.float32)
    rope = np.concatenate([cos, cos, -sin, sin], axis=0)
    slopes = (2.0 ** (-8.0 * np.arange(1, 19, dtype=np.float32) / 18)).astype(np.float32)
    s_hi = bf(slopes)
    s_lo = bf(slopes - s_hi)
    a = (t // 64).astype(np.float32)
    b = (t % 64).astype(np.float32)
    one = np.ones(S, np.float32)
    qc = np.zeros((18, 8, S), np.float32)
    kc = np.zeros((18, 8, S), np.float32)
    for h in range(18):
        qc[h] = np.stack([-512 * a, -8 * b, -512 * a, -8 * b, s_hi[h] * one, s_hi[h] * one, s_lo[h] * one, s_lo[h] * one])
        kc[h] = np.stack([s_hi[h] * one, s_hi[h] * one, s_lo[h] * one, s_lo[h] * one, 512 * a, 8 * b, 512 * a, 8 * b])
    onehot = (t[None, :] // 256 == np.arange(16)[:, None]).astype(np.float32)
    NEG = np.float32(-30000.0)
    k = np.arange(128)[:, None]
    c = np.arange(896)[None, :]
    cmask = np.where(c - 384 >= k, np.float32(0), NEG).astype(np.float32)
    q = np.arange(128)[None, :]
    own = np.where(k <= q, np.float32(0), NEG).astype(np.float32)
    prev = np.where(k >= q, np.float32(0), NEG).astype(np.float32)
    dmask = np.stack([np.tile(own, (1, 4)), np.tile(prev, (1, 4))], axis=1)
    qt_i = np.arange(512) // 16
    n_i = np.arange(512) % 16
    bo = qt_i // 2
    selc = np.stack([(n_i < bo).astype(np.float32), np.where(n_i < bo, np.float32(0), np.float32(-1e30)),
                     (n_i != bo).astype(np.float32)], axis=0)
    selc = np.ascontiguousarray(np.broadcast_to(selc[None], (128, 3, 512))).astype(np.float32)
    return {"c_selc": selc, "c_rope": np.ascontiguousarray(rope), "c_qc": qc, "c_kc": kc, "c_onehot": onehot, "c_cmask": cmask,
            "c_dmask": np.ascontiguousarray(dmask), "c_ident": np.eye(128, dtype=np.float32)}


def host_layout(inputs):
    f = lambda a: np.ascontiguousarray(np.asarray(a, dtype=np.float32))
    col = lambda v: f(v).reshape(-1, 128).T
    gains = np.zeros((128, 124), np.float32)
    for l in range(NL):
        gains[:, l * 8:(l + 1) * 8] = col(inputs["ffn1_norm"][l])
        gains[:, 32 + l * 8:32 + (l + 1) * 8] = col(inputs["mix_norm"][l])
        gains[:, 72 + l * 8:72 + (l + 1) * 8] = col(inputs["ffn2_norm"][l])
        gains[:, 64 + l * 2:64 + (l + 1) * 2] = col(inputs["q_norm"][l])
        gains[:, 112 + l:113 + l] = col(inputs["kv_norm"][l])
    gains[:, 104:112] = col(inputs["final_norm"])
    shared = {"gains": gains}
    shared.update(host_consts())
    for nm in ("ffn1_w_gate", "ffn1_w_up", "ffn1_w_down", "ffn2_w_gate", "ffn2_w_up", "ffn2_w_down", "w_in", "w_uq",
               "w_ukv", "w_br_mla", "w_br_moba", "w_br_dil", "w_out"):
        shared[nm] = f(inputs[nm])
    x = np.asarray(inputs["x"], dtype=np.float32)
    maps = []
    for b in range(NCORES):
        m = dict(shared)
        m["xT"] = np.ascontiguousarray(x[b].T)
        maps.append(m)
    return maps


_NC_CACHE = {}


def kernel(**inputs):
    if "nc" not in _NC_CACHE:
        _NC_CACHE["nc"] = Builder().build()
    nc = _NC_CACHE["nc"]
    maps = host_layout(inputs)
    res = run_bass_kernel_spmd(nc, maps, core_ids=list(range(NCORES)))
    out = np.stack([np.ascontiguousarray(res.results[b]["outT"].T) for b in range(NCORES)], axis=0)
    return out.astype(np.float32)
```
